# Optimizing a Trainium2 kernel written in Bass

```python
import math
import jax, jax.numpy as jnp
from jax import lax
import numpy as np


D_MODEL = 1024
BATCH = 8
SEQ = 8192
DEPTH = 1

MIX_WIDTH = D_MODEL
NSA_WIDTH = MIX_WIDTH // 2
SSD_WIDTH = MIX_WIDTH - NSA_WIDTH

NSA_HEAD_DIM = 64
NSA_HEADS = NSA_WIDTH // NSA_HEAD_DIM
NSA_KV_GROUPS = 2
NSA_HPG = NSA_HEADS // NSA_KV_GROUPS
NSA_KV_WIDTH = NSA_KV_GROUPS * NSA_HEAD_DIM
N_BRANCH = 3
CMP_BLOCK = 32
CMP_STRIDE = 16
CMP_HIDDEN = 256
SEL_BLOCK = 64
SEL_TOPN = 16
WINDOW = 512
Q_BLOCK = 128
ROPE_THETA = 500000.0
ROPE_DIM = NSA_HEAD_DIM // 4

SSD_HEAD_DIM = 64
SSD_HEADS = SSD_WIDTH // SSD_HEAD_DIM
SSD_GROUPS = 2
SSD_RPG = SSD_HEADS // SSD_GROUPS
SSD_STATE = 128
SSD_CONV = 4
SSD_CHUNK = 128
SSD_CONV_CH = SSD_WIDTH + 2 * SSD_GROUPS * SSD_STATE

PEER_HEADS = 8
PEER_NKEYS = 128
PEER_EXPERTS = PEER_NKEYS * PEER_NKEYS
PEER_QDIM = 256
PEER_TOPK = 16
PEER_TOKEN_BLOCK = 256

IN_SPLIT_SIZES = (NSA_WIDTH,) + (NSA_KV_WIDTH,) * 6 + (NSA_HEADS * N_BRANCH, SSD_WIDTH, SSD_CONV_CH, SSD_HEADS)
IN_COLS = sum(IN_SPLIT_SIZES)

NORM_EPS = 1e-6
NEG_INF = -1e30
FORCE_BONUS = 1e4

kernel_name = 'hymba_nsa_ssd_peer_adaln_block'


def rmsnorm(x, w):
    xf = x.astype(jnp.float32)
    y = xf * lax.rsqrt(jnp.mean(xf * xf, axis=-1, keepdims=True) + NORM_EPS)
    return (y * w.astype(jnp.float32)).astype(x.dtype)


def modulate(x, w, shift, scale):
    return rmsnorm(x, w) * (1 + scale[:, None, :]) + shift[:, None, :]


def masked_softmax(s, mask):
    s = jnp.where(mask, s.astype(jnp.float32), NEG_INF)
    p = jax.nn.softmax(s, axis=-1)
    return jnp.where(mask, p, 0.0)


def partial_rope(x, pos):
    half = ROPE_DIM // 2
    inv_freq = ROPE_THETA ** (-jnp.arange(half, dtype=jnp.float32) / half)
    ang = pos.astype(jnp.float32)[:, None] * inv_freq[None, :]
    cos, sin = jnp.cos(ang), jnp.sin(ang)
    xr = x[..., :ROPE_DIM].astype(jnp.float32)
    x1, x2 = xr[..., :half], xr[..., half:]
    rot = jnp.concatenate([x1 * cos - x2 * sin, x2 * cos + x1 * sin], axis=-1).astype(x.dtype)
    return jnp.concatenate([rot, x[..., ROPE_DIM:]], axis=-1)


def nsa_mixer(q, k_cmp, v_cmp, k_sel, v_sel, k_win, v_win, gate_logits,
              pe_k, pe_v, w1_k, w2_k, w1_v, w2_v):
    bsz, s, _ = q.shape
    dh = NSA_HEAD_DIM
    G, R = NSA_KV_GROUPS, NSA_HPG
    pos = jnp.arange(s)
    scale = NSA_HEAD_DIM ** -0.5

    qh = q.reshape(bsz, s, G, R, dh).transpose(0, 2, 3, 1, 4)
    qr = partial_rope(qh, pos)

    def heads_kv(t):
        return t.reshape(bsz, s, G, dh).transpose(0, 2, 1, 3)

    k_cmp, v_cmp, k_sel, v_sel, k_win, v_win = (heads_kv(t) for t in (k_cmp, v_cmp, k_sel, v_sel, k_win, v_win))
    k_sel = partial_rope(k_sel, pos)
    k_win = partial_rope(k_win, pos)
    gates = jax.nn.sigmoid(gate_logits.astype(jnp.float32)).astype(q.dtype)
    gates = gates.reshape(bsz, s, G, R, N_BRANCH).transpose(0, 2, 3, 1, 4)

    n_cmp = (s - CMP_BLOCK) // CMP_STRIDE + 1
    cmp_start = jnp.arange(n_cmp) * CMP_STRIDE
    cmp_idx = cmp_start[:, None] + jnp.arange(CMP_BLOCK)[None, :]

    def compress(kv, pe, w1, w2):
        blk = (kv[:, :, cmp_idx] + pe).reshape(bsz, G, n_cmp, CMP_BLOCK * dh)
        return jax.nn.gelu(blk @ w1) @ w2

    kc = compress(k_cmp, pe_k, w1_k, w2_k)
    vc = compress(v_cmp, pe_v, w1_v, w2_v)
    cmp_end = cmp_start + CMP_BLOCK - 1

    n_sel = s // SEL_BLOCK
    n_top = min(SEL_TOPN, n_sel)
    sel_start = jnp.arange(n_sel) * SEL_BLOCK
    overlap = jnp.maximum(
        jnp.minimum(cmp_start[:, None] + CMP_BLOCK, sel_start[None, :] + SEL_BLOCK)
        - jnp.maximum(cmp_start[:, None], sel_start[None, :]), 0).astype(jnp.float32) / CMP_BLOCK
    ks_blk = k_sel.reshape(bsz, G, n_sel, SEL_BLOCK, dh)
    vs_blk = v_sel.reshape(bsz, G, n_sel, SEL_BLOCK, dh)
    gather_blocks = jax.vmap(jax.vmap(lambda blocks, idx: blocks[idx]))

    kw_pad = jnp.pad(k_win, ((0, 0), (0, 0), (WINDOW, 0), (0, 0)))
    vw_pad = jnp.pad(v_win, ((0, 0), (0, 0), (WINDOW, 0), (0, 0)))
    sel_off = jnp.arange(SEL_BLOCK)
    jj = jnp.arange(n_sel)

    def query_block(qb):
        q0 = qb * Q_BLOCK
        t = q0 + jnp.arange(Q_BLOCK)
        qc = lax.dynamic_slice_in_dim(qh, q0, Q_BLOCK, axis=3)
        qrc = lax.dynamic_slice_in_dim(qr, q0, Q_BLOCK, axis=3)
        g = lax.dynamic_slice_in_dim(gates, q0, Q_BLOCK, axis=3)

        s_c = jnp.einsum('bgrtd,bgnd->bgrtn', qc, kc) * scale
        p_c = masked_softmax(s_c, cmp_end[None, :] <= t[:, None])
        o_c = jnp.einsum('bgrtn,bgnd->bgrtd', p_c.astype(vc.dtype), vc)

        imp = jnp.einsum('bgrtn,nj->bgtj', p_c, overlap)
        blk_t = t // SEL_BLOCK
        forced = (jj[None, :] == 0) | (jj[None, :] == blk_t[:, None]) | (jj[None, :] == blk_t[:, None] - 1)
        imp = jnp.where(sel_start[None, :] <= t[:, None], imp + jnp.where(forced, FORCE_BONUS, 0.0), NEG_INF)
        _, sel = lax.top_k(imp, n_top)

        kg = gather_blocks(ks_blk, sel).reshape(bsz, G, Q_BLOCK, n_top * SEL_BLOCK, dh)
        vg = gather_blocks(vs_blk, sel).reshape(bsz, G, Q_BLOCK, n_top * SEL_BLOCK, dh)
        kpos = (sel[..., None] * SEL_BLOCK + sel_off).reshape(bsz, G, Q_BLOCK, n_top * SEL_BLOCK)
        s_s = jnp.einsum('bgrtd,bgtmd->bgrtm', qrc, kg) * scale
        p_s = masked_softmax(s_s, (kpos <= t[None, None, :, None])[:, :, None])
        o_s = jnp.einsum('bgrtm,bgtmd->bgrtd', p_s.astype(vg.dtype), vg)

        kw = lax.dynamic_slice_in_dim(kw_pad, q0, Q_BLOCK + WINDOW, axis=2)
        vw = lax.dynamic_slice_in_dim(vw_pad, q0, Q_BLOCK + WINDOW, axis=2)
        kpos_w = q0 - WINDOW + jnp.arange(Q_BLOCK + WINDOW)
        m_w = (kpos_w[None, :] >= 0) & (kpos_w[None, :] <= t[:, None]) & (t[:, None] - kpos_w[None, :] < WINDOW)
        s_w = jnp.einsum('bgrtd,bgkd->bgrtk', qrc, kw) * scale
        p_w = masked_softmax(s_w, m_w)
        o_w = jnp.einsum('bgrtk,bgkd->bgrtd', p_w.astype(vw.dtype), vw)

        return g[..., 0:1] * o_c + g[..., 1:2] * o_s + g[..., 2:3] * o_w

    out = lax.map(query_block, jnp.arange(s // Q_BLOCK))
    out = out.transpose(1, 2, 3, 0, 4, 5).reshape(bsz, G, R, s, dh)
    return out.transpose(0, 3, 1, 2, 4).reshape(bsz, s, NSA_WIDTH)


def causal_dwconv(x, w, b):
    k = w.shape[0]
    y = lax.conv_general_dilated(x, w[:, None, :].astype(x.dtype), window_strides=(1,), padding=[(k - 1, 0)],
                                 dimension_numbers=('NWC', 'WIO', 'NWC'), feature_group_count=x.shape[-1])
    return y + b


def ssd_scan(xs, dt, a, bm, cm):
    bsz, s = xs.shape[:2]
    L = SSD_CHUNK
    nc = s // L
    G, R, P, N = SSD_GROUPS, SSD_RPG, SSD_HEAD_DIM, SSD_STATE
    X = (xs * dt[..., None]).reshape(bsz, nc, L, G, R, P)
    A = (dt * a).reshape(bsz, nc, L, G, R).transpose(0, 3, 4, 1, 2)
    Bc = bm.reshape(bsz, nc, L, G, N)
    Cc = cm.reshape(bsz, nc, L, G, N)
    A_cs = jnp.cumsum(A, axis=-1)
    causal = jnp.tril(jnp.ones((L, L), dtype=bool))
    Lm = jnp.exp(jnp.where(causal, A_cs[..., :, None] - A_cs[..., None, :], -jnp.inf))
    CB = jnp.einsum('bclgn,bcsgn->bgcls', Cc, Bc)
    Y_diag = jnp.einsum('bgrcls,bcsgrp->bclgrp', CB[:, :, None] * Lm, X)
    decay_states = jnp.exp(A_cs[..., -1:] - A_cs)
    states = jnp.einsum('bclgn,bgrcl,bclgrp->bcgrpn', Bc, decay_states, X)
    chunk_decay = jnp.exp(A_cs[..., -1])

    def step(h, inp):
        st, d = inp
        return h * d[..., None, None] + st, h

    h0 = jnp.zeros((bsz, G, R, P, N), jnp.float32)
    _, prev = lax.scan(step, h0, (states.transpose(1, 0, 2, 3, 4, 5), chunk_decay.transpose(3, 0, 1, 2)))
    prev = prev.transpose(1, 0, 2, 3, 4, 5)
    Y_off = jnp.einsum('bclgn,bcgrpn,bgrcl->bclgrp', Cc, prev, jnp.exp(A_cs))
    return (Y_diag + Y_off).reshape(bsz, s, G, R, P)


def ssd_mixer(z, xbc, dt_raw, conv_w, conv_b, dt_bias, a_log, d_skip, norm_w):
    bsz, s, _ = z.shape
    f32 = jnp.float32
    xbc = jax.nn.silu(causal_dwconv(xbc, conv_w, conv_b)).astype(f32)
    xs, bm, cm = jnp.split(xbc, [SSD_WIDTH, SSD_WIDTH + SSD_GROUPS * SSD_STATE], axis=-1)
    xs = xs.reshape(bsz, s, SSD_GROUPS, SSD_RPG, SSD_HEAD_DIM)
    bm = bm.reshape(bsz, s, SSD_GROUPS, SSD_STATE)
    cm = cm.reshape(bsz, s, SSD_GROUPS, SSD_STATE)
    dt = jax.nn.softplus(dt_raw.astype(f32) + dt_bias.astype(f32)).reshape(bsz, s, SSD_GROUPS, SSD_RPG)
    a = -jnp.exp(a_log.astype(f32)).reshape(SSD_GROUPS, SSD_RPG)
    y = ssd_scan(xs, dt, a, bm, cm) + xs * d_skip.astype(f32).reshape(SSD_GROUPS, SSD_RPG)[:, :, None]
    y = y.reshape(bsz, s, SSD_WIDTH) * jax.nn.silu(z.astype(f32))
    return rmsnorm(y, norm_w).astype(z.dtype)


def peer_ffn(h, w_q, q_norm_w, sub_keys, expert_down, expert_up):
    bsz, s, d = h.shape
    n_tok = bsz * s
    tb = math.gcd(n_tok, PEER_TOKEN_BLOCK)
    half = PEER_QDIM // 2

    def token_block(xb):
        q = rmsnorm((xb @ w_q).reshape(tb, PEER_HEADS, PEER_QDIM), q_norm_w)
        q = q.reshape(tb, PEER_HEADS, 2, half)
        sc = jnp.einsum('thkd,hknd->thkn', q, sub_keys)
        s1, i1 = lax.top_k(sc[:, :, 0], PEER_TOPK)
        s2, i2 = lax.top_k(sc[:, :, 1], PEER_TOPK)
        cand_s = (s1[..., :, None] + s2[..., None, :]).reshape(tb, PEER_HEADS, PEER_TOPK * PEER_TOPK)
        cand_i = (i1[..., :, None] * PEER_NKEYS + i2[..., None, :]).reshape(tb, PEER_HEADS, PEER_TOPK * PEER_TOPK)
        top_s, top_pos = lax.top_k(cand_s, PEER_TOPK)
        eidx = jnp.take_along_axis(cand_i, top_pos, axis=-1)
        gate = jax.nn.softmax(top_s.astype(jnp.float32), axis=-1)
        u = expert_down[eidx]
        v = expert_up[eidx]
        act = jax.nn.gelu(jnp.einsum('td,thkd->thk', xb, u))
        return jnp.einsum('thk,thkd->td', (gate * act).astype(v.dtype), v)

    y = lax.map(token_block, h.reshape(n_tok // tb, tb, d))
    return y.reshape(bsz, s, d)


def setup_inputs(seed: int = 0) -> dict:
    key = jax.random.key(seed)
    ks = jax.random.split(key, 32)
    f32 = jnp.float32
    L = DEPTH

    def nrm(k, shape, scale):
        return jax.random.normal(k, shape, f32) * scale

    dt0 = jnp.exp(jax.random.uniform(ks[17], (L, SSD_HEADS), f32, math.log(1e-3), math.log(1e-1)))
    return {
        'x': nrm(ks[0], (BATCH, SEQ, D_MODEL), 1.0),
        'c': nrm(ks[1], (BATCH, D_MODEL), 1.0),
        'w_ada': nrm(ks[2], (L, D_MODEL, 6 * D_MODEL), 0.5 * D_MODEL ** -0.5),
        'b_ada': nrm(ks[3], (L, 6 * D_MODEL), 0.02),
        'norm1_w': 1.0 + nrm(ks[4], (L, D_MODEL), 0.02),
        'w_in': nrm(ks[5], (L, D_MODEL, IN_COLS), D_MODEL ** -0.5),
        'cmp_pe_k': nrm(ks[6], (L, CMP_BLOCK, NSA_HEAD_DIM), 0.1),
        'cmp_pe_v': nrm(ks[7], (L, CMP_BLOCK, NSA_HEAD_DIM), 0.1),
        'cmp_w1_k': nrm(ks[8], (L, CMP_BLOCK * NSA_HEAD_DIM, CMP_HIDDEN), (CMP_BLOCK * NSA_HEAD_DIM) ** -0.5),
        'cmp_w2_k': nrm(ks[9], (L, CMP_HIDDEN, NSA_HEAD_DIM), CMP_HIDDEN ** -0.5),
        'cmp_w1_v': nrm(ks[10], (L, CMP_BLOCK * NSA_HEAD_DIM, CMP_HIDDEN), (CMP_BLOCK * NSA_HEAD_DIM) ** -0.5),
        'cmp_w2_v': nrm(ks[11], (L, CMP_HIDDEN, NSA_HEAD_DIM), CMP_HIDDEN ** -0.5),
        'nsa_norm_w': 1.0 + nrm(ks[12], (L, NSA_WIDTH), 0.02),
        'conv_w': nrm(ks[13], (L, SSD_CONV, SSD_CONV_CH), SSD_CONV ** -0.5),
        'conv_b': nrm(ks[14], (L, SSD_CONV_CH), 0.02),
        'dt_bias': dt0 + jnp.log(-jnp.expm1(-dt0)),
        'a_log': jnp.log(jax.random.uniform(ks[15], (L, SSD_HEADS), f32, 1.0, 16.0)),
        'd_skip': 1.0 + nrm(ks[16], (L, SSD_HEADS), 0.02),
        'ssd_norm_w': 1.0 + nrm(ks[18], (L, SSD_WIDTH), 0.02),
        'w_out': nrm(ks[19], (L, MIX_WIDTH, D_MODEL), MIX_WIDTH ** -0.5),
        'norm2_w': 1.0 + nrm(ks[20], (L, D_MODEL), 0.02),
        'peer_wq': nrm(ks[21], (L, D_MODEL, PEER_HEADS * PEER_QDIM), D_MODEL ** -0.5),
        'peer_qnorm_w': 1.0 + nrm(ks[22], (L, PEER_QDIM), 0.02),
        'peer_sub_keys': nrm(ks[23], (L, PEER_HEADS, 2, PEER_NKEYS, PEER_QDIM // 2), (PEER_QDIM // 2) ** -0.5),
        'peer_down': nrm(ks[24], (L, PEER_EXPERTS, D_MODEL), D_MODEL ** -0.5),
        'peer_up': nrm(ks[25], (L, PEER_EXPERTS, D_MODEL), PEER_HEADS ** -0.5),
        'final_norm_w': 1.0 + nrm(ks[26], (D_MODEL,), 0.02),
    }


def reference(x, c, w_ada, b_ada, norm1_w, w_in, cmp_pe_k, cmp_pe_v, cmp_w1_k, cmp_w2_k, cmp_w1_v, cmp_w2_v,
              nsa_norm_w, conv_w, conv_b, dt_bias, a_log, d_skip, ssd_norm_w, w_out, norm2_w,
              peer_wq, peer_qnorm_w, peer_sub_keys, peer_down, peer_up, final_norm_w):
    split_at = np.cumsum(IN_SPLIT_SIZES)[:-1].tolist()
    for layer in range(DEPTH):
        mod = c @ w_ada[layer] + b_ada[layer]
        sh1, sc1, g1, sh2, sc2, g2 = jnp.split(mod, 6, axis=-1)

        h = modulate(x, norm1_w[layer], sh1, sc1)
        proj = h @ w_in[layer]
        q, kc, vc, ksel, vsel, kwin, vwin, gl, z, xbc, dtr = jnp.split(proj, split_at, axis=-1)
        o_nsa = rmsnorm(nsa_mixer(q, kc, vc, ksel, vsel, kwin, vwin, gl,
                                  cmp_pe_k[layer], cmp_pe_v[layer], cmp_w1_k[layer], cmp_w2_k[layer],
                                  cmp_w1_v[layer], cmp_w2_v[layer]), nsa_norm_w[layer])
        o_ssd = ssd_mixer(z, xbc, dtr, conv_w[layer], conv_b[layer], dt_bias[layer], a_log[layer],
                          d_skip[layer], ssd_norm_w[layer])
        mix = jnp.concatenate([o_nsa, o_ssd], axis=-1) @ w_out[layer]
        x = x + g1[:, None, :] * mix

        h2 = modulate(x, norm2_w[layer], sh2, sc2)
        x = x + g2[:, None, :] * peer_ffn(h2, peer_wq[layer], peer_qnorm_w[layer], peer_sub_keys[layer],
                                          peer_down[layer], peer_up[layer])
    return rmsnorm(x, final_norm_w)
```

```python
import numpy as np
import concourse.bass as bass
import concourse.mybir as mybir
from concourse.bass_utils import run_bass_kernel_spmd
from contextlib import ExitStack

F32 = mybir.dt.float32
BF16 = mybir.dt.bfloat16
ALU = mybir.AluOpType
AF = mybir.ActivationFunctionType
AX = mybir.AxisListType

D = 1024
NEG = -30000.0
EPS = 1e-6


class Buf:
    __slots__ = ("name", "lw", "rd", "ps")

    def __init__(self, name=""):
        self.name = name
        self.lw = None
        self.rd = []
        self.ps = False


class TT:
    def __init__(self, h, shape, name):
        self.h = h
        self.shape = list(shape)
        self.free = int(np.prod(shape[1:]))
        self.b = Buf(name)

    def __getitem__(self, k):
        return self.h[k]

    def ap(self, off, dims, p0=0, npart=128):
        return bass.AP(self.h, p0 * self.free + off, [[self.free, npart]] + [list(d) for d in dims])


class KB:
    def __init__(self, nc, es):
        self.nc = nc
        self.es = es
        self.engs = {"pe": nc.tensor, "act": nc.scalar, "dve": nc.vector, "pool": nc.gpsimd, "sp": nc.sync}
        self.sems = {}
        self.cnt = {}
        for e in self.engs:
            self.sems[e] = es.enter_context(nc.semaphore("s_" + e))
            self.cnt[e] = 0
        self.seen = {e: {} for e in self.engs}
        self.dpool = {}
        self.dnext = {}
        for q in ("sp", "pool", "act"):
            self.dpool[q] = []
            for i in range(16 if q == "sp" else 2):
                k = "d_%s%d" % (q, i)
                self.sems[k] = es.enter_context(nc.semaphore(k))
                self.cnt[k] = 0
                self.dpool[q].append(k)
            self.dnext[q] = 0
        self.ninst = 0

    def sb(self, name, shape, dt):
        return TT(self.es.enter_context(self.nc.sbuf_tensor(name, list(shape), dt)), shape, name)

    def ps(self, name, shape, dt):
        t = TT(self.es.enter_context(self.nc.psum_tensor(name, list(shape), dt)), shape, name)
        t.b.ps = True
        return t

    def _need(self, e, deps):
        need = {}
        for d in deps:
            if d is None:
                continue
            k, v, _ = d
            if self.seen[e].get(k, 0) >= v:
                continue
            if need.get(k, 0) < v:
                need[k] = v
        for k, v in need.items():
            self.engs[e].wait_ge(self.sems[k], v)
            self.seen[e][k] = v

    def _deps(self, e, reads, writes):
        deps = []
        for b in reads:
            if b.lw is not None:
                deps.append(b.lw)
            if b.ps:
                for r in b.rd:
                    if r[2] != e:
                        deps.append(r)
        for b in writes:
            if b.lw is not None:
                deps.append(b.lw)
            for r in b.rd:
                if r[2] != e or r[0] != e:
                    deps.append(r)
        if e == "pe":
            deps = [d for d in deps if d[0] != "pe"]
        return deps

    def op(self, e, fn, reads=(), writes=()):
        reads = [getattr(r, "b", r) for r in reads]
        writes = [getattr(w, "b", w) for w in writes]
        self._need(e, self._deps(e, reads, writes))
        inst = fn(self.engs[e])
        self.cnt[e] += 1
        inst.then_inc(self.sems[e], 1)
        tok = (e, self.cnt[e], e)
        for b in reads:
            b.rd.append(tok)
            if len(b.rd) > 32:
                best = {}
                for k, v, en in b.rd:
                    if k not in best or best[k][1] < v:
                        best[k] = (k, v, en)
                b.rd = list(best.values())
        for b in writes:
            b.lw = tok
            b.rd = []
        self.ninst += 1
        return inst

    def dma(self, q, out, in_, reads=(), writes=(), **kw):
        reads = [getattr(r, "b", r) for r in reads]
        writes = [getattr(w, "b", w) for w in writes]
        self._need(q, self._deps(q, reads, writes))
        k = self.dpool[q][self.dnext[q] % len(self.dpool[q])]
        self.dnext[q] += 1
        if self.cnt[k] > 0:
            self._need(q, [(k, self.cnt[k], q)])
        inst = self.engs[q].dma_start(out=out, in_=in_, **kw)
        self.cnt[k] += 16
        inst.then_inc(self.sems[k], 16)
        tok = (k, self.cnt[k], q)
        for b in reads:
            b.rd.append(tok)
        for b in writes:
            b.lw = tok
            b.rd = []
        self.ninst += 1
        return tok

    def barrier(self):
        for e in self.engs:
            deps = [(k, v, "x") for k, v in self.cnt.items() if v > 0 and k != e]
            self._need(e, deps)

    def finish(self, bufs):
        deps = []
        for b in bufs:
            b = getattr(b, "b", b)
            if b.lw is not None:
                deps.append(b.lw)
        self._need("sp", deps)


def make_consts(S):
    NT = S // 128
    NSEL = S // 64
    p = np.arange(128)
    c = {}
    c["ident"] = np.eye(128, dtype=np.float32)
    c["tri"] = (p[:, None] <= p[None, :]).astype(np.float32)
    c["m1"] = (p[:, None] > p[None, :]).astype(np.float32)
    c["ones"] = np.ones((128, 128), np.float32)
    c["caus"] = np.where(p[:, None] <= p[None, :], 0.0, NEG).astype(np.float32)
    c["upper"] = np.where(p[:, None] > p[None, :], 0.0, NEG).astype(np.float32)
    cm = np.zeros((128, 17, 128), np.float32)
    for dl in range(17):
        cm[:, dl, :] = np.where(16 * p[:, None] - p[None, :] <= 128 * dl - 15, 0.0, NEG)
    c["cmask"] = cm.reshape(128, 17 * 128)
    NCT = max(1, S // 2048)
    n_cmp = S // 16 - 1
    ov = np.zeros((128, NCT, NSEL), np.float32)
    for n in range(n_cmp):
        for j in range(NSEL):
            o = min(16 * n + 32, 64 * j + 64) - max(16 * n, 64 * j)
            if o > 0:
                ov[(n + 1) % 128, (n + 1) // 128, j] = o / 32.0
    c["ov"] = ov.reshape(128, NCT * NSEL)
    c["jb"] = (np.arange(NSEL)[None, :] - (p[:, None] // 64)).astype(np.float32)
    col0 = np.zeros((128, NSEL), np.float32)
    col0[:, 0] = 1.0
    c["col0"] = col0
    half = 8
    inv = (500000.0 ** (-np.arange(half, dtype=np.float32) / half)).astype(np.float32)
    pos = np.arange(S, dtype=np.float32)
    ang = (pos[:, None] * inv[None, :]).astype(np.float32)
    rope = np.concatenate([np.cos(ang), np.sin(ang)], axis=1).astype(np.float32)
    c["rope"] = np.ascontiguousarray(rope.reshape(NT, 128, 16).transpose(1, 0, 2)).reshape(128, NT * 16)
    names = ["tri", "m1", "ones", "jb", "col0", "rope", "ident", "caus", "upper", "cmask", "ov"]
    offs = {}
    o = 0
    for n in names:
        offs[n] = (o, c[n].shape[1])
        o += c[n].shape[1]
    return np.concatenate([c[n] for n in names], axis=1).astype(np.float32), offs


C_Q, C_KC, C_VC, C_KS, C_VS, C_KW, C_VW, C_GL, C_Z, C_XBC, C_DT = 0, 512, 640, 768, 896, 1024, 1152, 1280, 1304, 1816, 2840


def build(S, dbg=False, STG=99, NTILES=None, SKIP=(), PEERON=True):
    NT = S // 128
    NSEL = S // 64
    NCT = max(1, S // 2048)
    TP = 256
    NP2 = S // TP
    cvals, coffs = make_consts(S)
    NCF = cvals.shape[1]
    nc = bass.Bass("TRN2", target_bir_lowering=False)
    dI = lambda n, s, dt=F32: nc.dram_tensor(n, list(s), dt, kind="ExternalInput").ap()
    x_d = dI("x", [S, D])
    cT_d = dI("cT", [128, 8])
    wada_d = dI("w_ada", [D, 6 * D])
    bada_d = dI("b_ada", [6 * D])
    n1_d = dI("norm1_w", [128, 8])
    n2_d = dI("norm2_w", [128, 8])
    fnw_d = dI("final_norm_w", [D])
    win_d = dI("w_in", [D, 2848])
    pek_d = dI("cmp_pe_k", [128, 16])
    pev_d = dI("cmp_pe_v", [128, 16])
    w1k_d = dI("cmp_w1_k", [2048, 256])
    w1v_d = dI("cmp_w1_v", [2048, 256])
    w2k_d = dI("cmp_w2_k", [256, 64])
    w2v_d = dI("cmp_w2_v", [256, 64])
    nsaw_d = dI("nsa_norm_w", [512])
    convw_d = dI("conv_w", [128, 8, 4])
    convb_d = dI("conv_b", [128, 8])
    dtb_d = dI("dt_bias", [8])
    alog_d = dI("a_log", [8])
    dsk_d = dI("d_skip", [512])
    ssdw_d = dI("ssd_norm_w", [512])
    wout_d = dI("w_out", [D, D])
    wq_d = dI("peer_wq", [D, 2048] if PEERON else [1, 1])
    qnw_d = dI("peer_qnorm_w", [128, 2])
    keys_d = dI("peer_keysT", [128, 16, 128] if PEERON else [1, 1, 1])
    down_d = dI("peer_downT", [128, 128, 8, 128] if PEERON else [1, 1, 1, 1])
    up_d = dI("peer_up", [128, 128, D] if PEERON else [1, 1, 1])
    const_d = dI("consts", [128, NCF])
    out_d = nc.dram_tensor("out", [S, D], F32, kind="ExternalOutput").ap()
    skind = "ExternalOutput" if dbg else "Internal"
    x1_d = nc.dram_tensor("x1s", [S, D], F32, kind=skind).ap()
    kselD = nc.dram_tensor("kselD", [NT, 64, 256], BF16, kind="Internal").ap()
    vselD = nc.dram_tensor("vselD", [NT, 128, 132], BF16, kind="Internal").ap()
    h2T_d = nc.dram_tensor("h2Ts", [NT, 128, 8 * 128], BF16, kind="Internal").ap()
    wg_d = nc.dram_tensor("wgs", [NT, 128, 128 * 128], BF16, kind="Internal").ap()
    dnb_d = nc.dram_tensor("dnbs", [128, 128, 8 * 128], BF16, kind="Internal").ap()
    upb_d = nc.dram_tensor("upbs", [128, 128, D], BF16, kind="Internal").ap()
    if dbg:
        dbg_d = nc.dram_tensor("dbg", [NT, 128, 2048], F32, kind="ExternalOutput").ap()

    with ExitStack() as es:
        kb = KB(nc, es)
        op, dma = kb.op, kb.dma
        DR = {n: Buf(n) for n in ("x1", "h2T", "wg", "dnb", "upb", "out", "dbg", "kselD", "vselD")}
        psf = [kb.ps("psf%d" % i, [128, 512], F32) for i in range(6)]
        psb = [kb.ps("psb%d" % i, [128, 1024], BF16) for i in range(2)]
        RES = ("tri", "m1", "ones", "jb", "col0", "rope")
        r_lo = coffs["tri"][0]
        r_hi = coffs["rope"][0] + coffs["rope"][1]
        NRES = r_hi - r_lo
        cst = kb.sb("cst", [128, NRES], F32)

        def cfo(name):
            return coffs[name][0] - r_lo

        def cf(name, lo=0, n=None):
            o, w = coffs[name]
            n = w - lo if n is None else n
            return cst[:, o - r_lo + lo:o - r_lo + lo + n]

        identb = kb.sb("identb", [128, 128], BF16)
        modfm = kb.sb("modfm", [128, 48], F32)
        gb = kb.sb("gb", [128, 2, D], F32)
        msc = kb.sb("msc", [128, 16], F32)
        es_mixp = ExitStack()
        kb.es = es_mixp
        cmaskb = kb.sb("cmaskb", [128, 17 * 128], BF16)
        Ekt = [kb.sb("Ekt%d" % i_, [128, 128], BF16) for i_ in range(4)]
        caus4 = kb.sb("caus4", [128, 512], BF16)
        upper4 = kb.sb("upper4", [128, 512], BF16)
        winb = kb.sb("winb", [128, 8, 2848], BF16)
        woutb = kb.sb("woutb", [128, 8, D], BF16)
        w1b = [kb.sb("w1b%d" % i, [128, 16, 256], BF16) for i in range(2)]
        w2b = [kb.sb("w2b%d" % i, [128, 2, 64], BF16) for i in range(2)]
        wcmp = kb.sb("wcmp", [128, 8, 4, 128], BF16)
        cbias = kb.sb("cbias", [128, 4], F32)
        prm = kb.sb("prm", [128, 1536 + 32], F32)
        cw = kb.sb("cw", [128, 8, 4], F32)
        cbs = kb.sb("cbs", [128, 8], F32)
        KWR = 5
        kselT = kb.sb("kselT", [64, 2, 128], BF16)
        kring = [kb.sb("kring%d" % i_, [64, 128], BF16) for i_ in range(4)]
        vring = [kb.sb("vring%d" % i_, [128, 66], BF16) for i_ in range(4)]
        kwinR = kb.sb("kwinR", [64, 2, KWR * 128], BF16)
        vsel = kb.sb("vsel", [128, 1, 2, 66], BF16)
        vwinR = kb.sb("vwinR", [128, KWR, 2, 66], BF16)
        kcT = kb.sb("kcT", [64, 2, NCT * 128], BF16)
        vcT = kb.sb("vcT", [64, 2, NCT * 128], BF16)
        VW = 64 + NSEL + 1
        vcx = kb.sb("vcx", [128, NCT, 2, VW + 1], BF16)
        stk = [kb.sb("stk%d" % i, [128, 2, 144], BF16) for i in range(2)]
        xbcp = kb.sb("xbcp", [128, 8, 131], F32)
        hst = kb.sb("hst", [128, 512], F32)
        hstb = kb.sb("hstb", [128, 512], BF16)

        es_setup = ExitStack()
        kb.es = es_setup
        dma("sp", cst[:], const_d[:, r_lo:r_hi], writes=[cst])
        stg = [kb.sb("stg%d" % i, [128, 3072], F32) for i in range(2)]
        stgi = [0]

        def load_cast(dst_tt, dst_ap_fn, src_ap_fn, nchunks, width, eng="pool"):
            for ci in range(nchunks):
                s_ = stg[stgi[0] % 2]
                stgi[0] += 1
                dma("sp", s_[:, 0:width], src_ap_fn(ci), writes=[s_])
                op(eng, lambda e, s_=s_, ci=ci: e.tensor_copy(out=dst_ap_fn(ci), in_=s_[:, 0:width]), [s_], [dst_tt])

        def cload(name):
            o, w = coffs[name]
            return const_d[:, o:o + w]
        load_cast(identb, lambda ci: identb[:], lambda ci: cload("ident"), 1, 128, "dve")
        load_cast(cmaskb, lambda ci: cmaskb[:], lambda ci: cload("cmask"), 1, 17 * 128, "dve")
        for nm_, dst_ in (("caus", caus4), ("upper", upper4)):
            s_ = stg[stgi[0] % 2]
            stgi[0] += 1
            dma("sp", s_[:, 0:128], cload(nm_), writes=[s_])
            op("dve", lambda e, s_=s_, dst_=dst_: e.tensor_copy(out=dst_[:].rearrange("p (r t) -> p r t", t=128), in_=s_.ap(0, [[0, 4], [1, 128]])), [s_], [dst_])
        cTs = kb.sb("cTs", [128, 8], F32)
        dma("sp", cTs[:], cT_d[:, :], writes=[cTs])
        crep = kb.sb("crep", [128, 8, 128], F32)
        for kc in range(8):
            op("dve", lambda e, kc=kc: e.tensor_copy(out=crep[:, kc, :], in_=cTs[:, kc:kc + 1].to_broadcast([128, 128])), [cTs], [crep])
        badafm = kb.sb("badafm", [128, 48], F32)
        dma("sp", badafm[:], bada_d.rearrange("(c p) -> p c", p=128), writes=[badafm], allow_slow_non_contiguous=True)
        badab = kb.sb("badab", [128, 2, D], F32)
        dma("sp", badab[:, 0, :], bada_d[2 * D:3 * D].partition_broadcast(128), writes=[badab])
        dma("sp", badab[:, 1, :], bada_d[5 * D:6 * D].partition_broadcast(128), writes=[badab])
        for part in range(2):
            for kc in range(8):
                w_ = stg[stgi[0] % 2]
                stgi[0] += 1
                dma("sp", w_[:, 0:3072], wada_d[kc * 128:(kc + 1) * 128, part * 3072:(part + 1) * 3072], writes=[w_])
                for ol in range(24):
                    oc = part * 24 + ol
                    op("pe", lambda e, w_=w_, oc=oc, ol=ol, kc=kc: e.matmul(psf[0][:, oc:oc + 1], lhsT=w_[:, ol * 128:(ol + 1) * 128], rhs=cTs[:, kc:kc + 1], start=(kc == 0 and ol == 0), stop=(kc == 7)), [w_, cTs], [psf[0]])
                gi = part
                for hh in range(2):
                    op("pe", lambda e, w_=w_, gi=gi, hh=hh, kc=kc: e.matmul(psf[1 + gi * 2 + hh][:, :], lhsT=crep[:, kc, :], rhs=w_[:, 2048 + hh * 512:2048 + hh * 512 + 512], start=(kc == 0), stop=(kc == 7)), [w_, crep], [psf[1 + gi * 2 + hh]])
        op("dve", lambda e: e.tensor_tensor(out=modfm[:], in0=psf[0][:, 0:48], in1=badafm[:], op=ALU.add), [psf[0], badafm], [modfm])
        for gi in range(2):
            for hh in range(2):
                op("dve", lambda e, gi=gi, hh=hh: e.tensor_tensor(out=gb[:, gi, hh * 512:(hh + 1) * 512], in0=psf[1 + gi * 2 + hh][:, :], in1=badab[:, gi, hh * 512:(hh + 1) * 512], op=ALU.add), [psf[1 + gi * 2 + hh], badab], [gb])
        nws = kb.sb("nws", [128, 16], F32)
        dma("sp", nws[:, 0:8], n1_d[:, :], writes=[nws])
        dma("sp", nws[:, 8:16], n2_d[:, :], writes=[nws])
        op("dve", lambda e: e.scalar_tensor_tensor(out=msc[:, 0:8], in0=modfm[:, 8:16], scalar=1.0, in1=nws[:, 0:8], op0=ALU.add, op1=ALU.mult), [modfm, nws], [msc])
        op("dve", lambda e: e.scalar_tensor_tensor(out=msc[:, 8:16], in0=modfm[:, 32:40], scalar=1.0, in1=nws[:, 8:16], op0=ALU.add, op1=ALU.mult), [modfm, nws], [msc])
        load_cast(winb, lambda ci: winb[:, ci, :], lambda ci: win_d[ci * 128:(ci + 1) * 128, :], 8, 2848)
        load_cast(woutb, lambda ci: woutb[:, ci, :], lambda ci: wout_d[ci * 128:(ci + 1) * 128, :], 8, D)
        for i_, wd in enumerate((w1k_d, w1v_d)):
            wv_ = wd.rearrange("(j p) h -> p j h", p=128)
            for hf in range(2):
                load_cast(w1b[i_], lambda ci, i_=i_, hf=hf: w1b[i_][:, hf * 8:(hf + 1) * 8, :], lambda ci, wv_=wv_, hf=hf: wv_[:, hf * 8:(hf + 1) * 8, :], 1, 2048)
        for i_, wd in enumerate((w2k_d, w2v_d)):
            load_cast(w2b[i_], lambda ci, i_=i_: w2b[i_][:, :, :], lambda ci, wd=wd: wd.rearrange("(c p) d -> p c d", p=128), 1, 128)
        for kv, c0 in enumerate((C_KC, C_VC)):
            for g in range(2):
                for hf in range(2):
                    op("pool", lambda e, kv=kv, g=g, hf=hf, c0=c0: e.tensor_copy(out=wcmp[:, :, kv * 2 + g, hf * 64:(hf + 1) * 64], in_=winb[:, :, c0 + g * 64:c0 + (g + 1) * 64]), [winb], [wcmp])
        pes = kb.sb("pes", [128, 2, 16], F32)
        dma("sp", pes[:, 0, :], pek_d[:, :], writes=[pes])
        dma("sp", pes[:, 1, :], pev_d[:, :], writes=[pes])
        peb = kb.sb("peb", [128, 2, 16], BF16)
        op("dve", lambda e: e.tensor_copy(out=peb[:], in_=pes[:]), [pes], [peb])
        for kv in range(2):
            for hc in range(2):
                for j in range(16):
                    op("pe", lambda e, kv=kv, hc=hc, j=j: e.matmul(psf[5][:, kv * 2 + hc:kv * 2 + hc + 1], lhsT=w1b[kv][:, j, hc * 128:(hc + 1) * 128], rhs=peb[:, kv, j:j + 1], start=(j == 0), stop=(j == 15)), [w1b[kv], peb], [psf[5]])
        op("dve", lambda e: e.tensor_copy(out=cbias[:], in_=psf[5][:, 0:4]), [psf[5]], [cbias])
        dma("sp", prm[:, 0:512], nsaw_d.partition_broadcast(128), writes=[prm])
        dma("sp", prm[:, 512:1024], ssdw_d.partition_broadcast(128), writes=[prm])
        dma("sp", prm[:, 1024:1536], dsk_d.partition_broadcast(128), writes=[prm])
        dma("sp", prm[:, 1536:1544], dtb_d.partition_broadcast(128), writes=[prm])
        dma("sp", prm[:, 1544:1552], alog_d.partition_broadcast(128), writes=[prm])
        op("act", lambda e: e.activation(out=prm[:, 1552:1560], in_=prm[:, 1544:1552], func=AF.Exp), [prm], [prm])
        op("dve", lambda e: e.tensor_scalar(out=prm[:, 1552:1560], in0=prm[:, 1552:1560], scalar1=-1.0, scalar2=None, op0=ALU.mult), [prm], [prm])
        NSAW, SSDW, DSK, DTB, AN = prm[:, 0:512], prm[:, 512:1024], prm[:, 1024:1536], prm[:, 1536:1544], prm[:, 1552:1560]
        dma("sp", cw[:], convw_d[:, :, :], writes=[cw])
        dma("sp", cbs[:], convb_d[:, :], writes=[cbs])
        for t_ in (kcT, vcT, vcx, vsel, vwinR, stk[0], stk[1], xbcp, hst, hstb):
            op("pool", lambda e, t_=t_: e.memset(t_[:], 0.0), [], [t_])
        for ct in range(NCT):
            s_ = stg[stgi[0] % 2]
            stgi[0] += 1
            o_ov = coffs["ov"][0]
            dma("sp", s_[:, 0:NSEL], const_d[:, o_ov + ct * NSEL:o_ov + (ct + 1) * NSEL], writes=[s_])
            for g in range(2):
                op("pool", lambda e, ct=ct, g=g, s_=s_: e.tensor_copy(out=vcx[:, ct, g, 64:64 + NSEL], in_=s_[:, 0:NSEL]), [s_], [vcx])
                op("pool", lambda e, ct=ct, g=g: e.memset(vcx[:, ct, g, 64 + NSEL:VW + 1], 1.0), [], [vcx])
        op("pool", lambda e: e.memset(vcx[0:1, 0, :, :], 0.0), [], [vcx])
        for g in range(2):
            op("pool", lambda e, g=g: e.memset(vsel[:, :, g, 64:66], 1.0), [], [vsel])
            op("pool", lambda e, g=g: e.memset(vwinR[:, :, g, 64:66], 1.0), [], [vwinR])
        kb.barrier()
        es_setup.close()
        es_mix = ExitStack()
        kb.es = es_mix
        xt = [kb.sb("xt%d" % i, [128, D], F32) for i in range(2)]
        junk = kb.sb("junk", [128, D], F32)
        st8 = kb.sb("st8", [128, 64], F32)
        xnb = kb.sb("xnb", [128, D], BF16)
        hT = kb.sb("hT", [128, 8, 128], BF16)
        tok = kb.sb("tok", [128, 1568], F32)
        TQ, TKS, TKW, TVS, TVW, TGL, TZ, TDT = 0, 512, 640, 768, 896, 1024, 1048, 1560
        qb = kb.sb("qb", [128, 2, 768], BF16)
        QT = kb.sb("QT", [64, 8, 128], BF16)
        QrT = kb.sb("QrT", [64, 8, 128], BF16)
        rtmp = kb.sb("rtmp", [128, 4, 12, 8], F32)
        gates = kb.sb("gates", [128, 24], F32)
        pT = [kb.sb("pT%d" % i, [128, 512], BF16) for i in range(2)]
        nselT = kb.sb("nselT", [128, 2, 512], BF16)
        impb = kb.sb("impb", [128, 6, NSEL], F32)
        nselb = kb.sb("nselb", [128, 128], BF16)
        m8 = kb.sb("m8", [128, 16], F32)
        coef = kb.sb("coef", [128, 3, 8], F32)
        onsa = kb.sb("onsa", [128, 512], F32)
        otmp = kb.sb("otmp", [128, 256], F32)
        ob = kb.sb("ob", [128, D], BF16)
        oT = kb.sb("oT", [128, 8, 128], BF16)
        hidT = kb.sb("hidT", [128, 64], BF16)
        xbcs = kb.sb("xbcs", [128, 8, 128], BF16)
        cacc = kb.sb("cacc", [128, 8, 128], F32)
        ctmp = kb.sb("ctmp", [128, 8, 128], F32)
        xstok = kb.sb("xstok", [128, 512], F32)
        Xd = kb.sb("Xd", [128, 512], BF16)
        Xdd = kb.sb("Xdd", [128, 512], BF16)
        Btok = kb.sb("Btok", [128, 2, 128], BF16)
        dts = kb.sb("dts", [128, 32], F32)
        atri = cacc
        cbm = kb.sb("cbm", [128, 2, 128], F32)
        lexp = kb.sb("lexp", [128, 4, 128], F32)
        cbl = kb.sb("cbl", [128, 8, 128], BF16)
        eab = ctmp
        cth = kb.sb("cth", [128, 8, 128], BF16)
        yss = kb.sb("yss", [128, 512], F32)
        zs = kb.sb("zs", [128, 512], F32)

        def rms_rstd(src_ap, n, dst_ap, src_bufs):
            op("act", lambda e: e.activation(out=junk[:, 0:n], in_=src_ap, func=AF.Square, accum_out=dst_ap), src_bufs, [junk, st8])
            op("dve", lambda e: e.tensor_scalar(out=dst_ap, in0=dst_ap, scalar1=1.0 / n, scalar2=EPS, op0=ALU.mult, op1=ALU.add), [st8], [st8])
            op("act", lambda e: e.activation(out=dst_ap, in_=dst_ap, func=AF.Sqrt), [st8], [st8])
            op("dve", lambda e: e.reciprocal(out=dst_ap, in_=dst_ap), [st8], [st8])

        def to_featT(src_bf, dstT, sc_ap_fn, sh_ap_fn, nch, src_bufs):
            for c in range(nch):
                op("pe", lambda e, c=c: e.transpose(out=psb[c // 8 % 2][:, (c % 8) * 128:(c % 8 + 1) * 128], in_=src_bf[:, c * 128:(c + 1) * 128], identity=identb[:]), src_bufs + [identb], [psb[c // 8 % 2]])
                if c % 8 == 7 or c == nch - 1:
                    c0 = c - c % 8
                    if sc_ap_fn is None:
                        op("act", lambda e, c0=c0, c=c: e.copy(out=dstT[:, c0:c + 1, :], in_=psb[c // 8 % 2][:, 0:(c - c0 + 1) * 128].rearrange("p (c t) -> p c t", t=128)), [psb[c // 8 % 2]], [dstT])
                    else:
                        for cc in range(c0, c + 1):
                            op("act", lambda e, cc=cc: e.activation(out=dstT[:, cc, :], in_=psb[cc // 8 % 2][:, (cc % 8) * 128:(cc % 8 + 1) * 128], func=AF.Identity, scale=sc_ap_fn(cc), bias=sh_ap_fn(cc)), [psb[cc // 8 % 2], msc, modfm], [dstT])

        def attn_tile(KT_ap, K_bufs, QTt, masks, V_ap, V_bufs, acc_ps, acc_w, first, last, si, starts=(0,)):
            ps_ = psf[si % 2]
            pt_ = pT[si % 2]
            nm = len(masks)
            op("pe", lambda e: e.matmul(ps_[:, :], lhsT=KT_ap, rhs=QTt, start=True, stop=(nm == 0)), K_bufs + [QT, QrT], [ps_])
            for mi, (ml, mr, mb) in enumerate(masks):
                if isinstance(mr, tuple):
                    for r in range(4):
                        op("pe", lambda e, ml=ml, mr=mr, mi=mi, r=r: e.matmul(ps_[:, r * 128:(r + 1) * 128], lhsT=ml, rhs=mr[1], start=False, stop=(mi == nm - 1)), mb, [ps_])
                else:
                    op("pe", lambda e, ml=ml, mr=mr, mi=mi: e.matmul(ps_[:, :], lhsT=ml, rhs=mr, start=False, stop=(mi == nm - 1)), mb, [ps_])
            op("act", lambda e: e.activation(out=pt_[:], in_=ps_[:, :], func=AF.Exp, scale=0.125), [ps_], [pt_])
            for r in range(4):
                op("pe", lambda e, r=r: e.matmul(acc_ps(r), lhsT=pt_[:, r * 128:(r + 1) * 128], rhs=V_ap, start=(first and r in starts), stop=last), [pt_] + V_bufs, acc_w)

        def rep4(tt, off):
            return tt.ap(off, [[0, 4], [1, 128]])

        for i in range(NT if NTILES is None else NTILES):
            xti = xt[i % 2]
            dma("sp", xti[:], x_d[i * 128:(i + 1) * 128, :], writes=[xti])
            rms_rstd(xti[:], D, st8[:, 0:1], [xti])
            op("dve", lambda e: e.tensor_scalar(out=xnb[:], in0=xti[:], scalar1=st8[:, 0:1], scalar2=None, op0=ALU.mult), [xti, st8], [xnb])
            to_featT(xnb, hT, lambda c: msc[:, c:c + 1], lambda c: modfm[:, c:c + 1], 8, [xnb])
            if STG < 1:
                dma("sp", x1_d[i * 128:(i + 1) * 128, :], xti[:], reads=[xti], writes=[DR["x1"]])
                continue
            if dbg and "dumph" in SKIP:
                op("dve", lambda e: e.tensor_copy(out=junk[:, :], in_=hT[:, :, :].rearrange("p c t -> p (c t)")), [hT], [junk])
                dma("sp", dbg_d[i, :, 0:1024], junk[:, :], reads=[junk], writes=[DR["dbg"]])
                op("dve", lambda e: e.tensor_copy(out=junk[:, :], in_=xnb[:, :]), [xnb], [junk])
                dma("sp", dbg_d[i, :, 1024:2048], junk[:, :], reads=[junk], writes=[DR["dbg"]])
            groups = [(C_Q, 512, TQ, 0), (C_KS, 128, TKS, 1), (C_KW, 128, TKW, 1), (C_VS, 128, TVS, 1), (C_VW, 128, TVW, 1),
                      (C_GL, 24, TGL, 2), (C_Z, 512, TZ, 3), (C_DT, 8, TDT, 2)]
            pso = {0: 0, 1: 0, 2: 0, 3: 0}
            for (c0, w, t0, bank) in groups:
                bk = psf[2 + bank]
                o_ = pso[bank]
                pso[bank] += w
                for kc in range(8):
                    op("pe", lambda e, kc=kc, c0=c0, w=w, o_=o_, bk=bk: e.matmul(bk[:, o_:o_ + w], lhsT=hT[:, kc, :], rhs=winb[:, kc, c0:c0 + w], start=(kc == 0), stop=(kc == 7)), [hT, winb], [bk])
            op("act", lambda e: e.copy(out=tok[:, TQ:TQ + 512], in_=psf[2][:, 0:512]), [psf[2]], [tok])
            op("dve", lambda e: e.tensor_copy(out=tok[:, TKS:TKS + 512], in_=psf[3][:, 0:512]), [psf[3]], [tok])
            op("dve", lambda e: e.tensor_copy(out=tok[:, TGL:TGL + 24], in_=psf[4][:, 0:24]), [psf[4]], [tok])
            op("dve", lambda e: e.tensor_copy(out=tok[:, TDT:TDT + 8], in_=psf[4][:, 24:32]), [psf[4]], [tok])
            op("act", lambda e: e.copy(out=tok[:, TZ:TZ + 512], in_=psf[5][:, 0:512]), [psf[5]], [tok])
            for kv in range(2):
                for g in range(2):
                    idx = kv * 2 + g
                    bk = psf[2 + idx % 2]
                    for kc in range(8):
                        op("pe", lambda e, kc=kc, idx=idx, bk=bk: e.matmul(bk[:, 0:128], lhsT=wcmp[:, kc, idx, :], rhs=hT[:, kc, :], start=(kc == 0), stop=(kc == 7)), [hT, wcmp], [bk])
                    op("act", lambda e, kv=kv, g=g, bk=bk: e.copy(out=stk[kv][0:64, g, 16:144], in_=bk[0:64, 0:128]), [bk], [stk[kv]])
                    op("dve", lambda e, kv=kv, g=g, bk=bk: e.tensor_copy(out=stk[kv][64:128, g, 14:142], in_=bk[64:128, 0:128]), [bk], [stk[kv]])
            for c in range(8):
                bk = psf[4 + c % 2]
                for kc in range(8):
                    op("pe", lambda e, kc=kc, c=c, bk=bk: e.matmul(bk[:, 0:128], lhsT=winb[:, kc, C_XBC + c * 128:C_XBC + (c + 1) * 128], rhs=hT[:, kc, :], start=(kc == 0), stop=(kc == 7)), [hT, winb], [bk])
                op("act" if c % 2 else "dve", (lambda e, c=c, bk=bk: e.copy(out=xbcp[:, c, 3:131], in_=bk[:, 0:128])) if c % 2 else (lambda e, c=c, bk=bk: e.tensor_copy(out=xbcp[:, c, 3:131], in_=bk[:, 0:128])), [bk], [xbcp])

            if STG < 2:
                dma("sp", x1_d[i * 128:(i + 1) * 128, :], xti[:], reads=[xti], writes=[DR["x1"]])
                continue
            op("dve", lambda e: e.tensor_copy(out=qb[:, 0, 0:512], in_=tok[:, TQ:TQ + 512]), [tok], [qb])
            op("pool", lambda e: e.tensor_copy(out=qb[:, 1, :], in_=tok[:, TQ:TQ + 768]), [tok], [qb])
            o_r = coffs["rope"][0] + i * 16
            cosv = cst.ap(cfo("rope") + i * 16, [[0, 12], [1, 8]])
            sinv = cst.ap(cfo("rope") + i * 16 + 8, [[0, 12], [1, 8]])
            x1v = tok.ap(TQ, [[64, 12], [1, 8]])
            x2v = tok.ap(TQ + 8, [[64, 12], [1, 8]])
            op("dve", lambda e: e.tensor_tensor(out=rtmp[:, 0], in0=x1v, in1=cosv, op=ALU.mult), [tok, cst], [rtmp])
            op("dve", lambda e: e.tensor_tensor(out=rtmp[:, 1], in0=x2v, in1=sinv, op=ALU.mult), [tok, cst], [rtmp])
            op("dve", lambda e: e.tensor_tensor(out=rtmp[:, 2], in0=x2v, in1=cosv, op=ALU.mult), [tok, cst], [rtmp])
            op("dve", lambda e: e.tensor_tensor(out=rtmp[:, 3], in0=x1v, in1=sinv, op=ALU.mult), [tok, cst], [rtmp])
            op("dve", lambda e: e.tensor_tensor(out=qb.ap(768, [[64, 12], [1, 8]]), in0=rtmp[:, 0], in1=rtmp[:, 1], op=ALU.subtract), [rtmp], [qb])
            op("dve", lambda e: e.tensor_tensor(out=qb.ap(768 + 8, [[64, 12], [1, 8]]), in0=rtmp[:, 2], in1=rtmp[:, 3], op=ALU.add), [rtmp], [qb])
            if STG < 1.3:
                dma("sp", x1_d[i * 128:(i + 1) * 128, :], xti[:], reads=[xti], writes=[DR["x1"]])
                continue
            for h in range(8):
                op("pe", lambda e, h=h: e.matmul(psf[2 + h // 4][0:64, (h % 4) * 128:(h % 4 + 1) * 128], lhsT=qb[:, 0, h * 64:(h + 1) * 64], rhs=identb[:], start=True, stop=True), [qb, identb], [psf[2 + h // 4]])
                op("pe", lambda e, h=h: e.matmul(psf[4 + h // 4][0:64, (h % 4) * 128:(h % 4 + 1) * 128], lhsT=qb[:, 1, h * 64:(h + 1) * 64], rhs=identb[:], start=True, stop=True), [qb, identb], [psf[4 + h // 4]])
            for hh_ in range(2):
                op("act", lambda e, hh_=hh_: e.copy(out=QT[:, hh_ * 4:(hh_ + 1) * 4, :], in_=psf[2 + hh_][0:64, :].rearrange("p (h t) -> p h t", t=128)), [psf[2 + hh_]], [QT])
                op("dve", lambda e, hh_=hh_: e.tensor_copy(out=QrT[:, hh_ * 4:(hh_ + 1) * 4, :], in_=psf[4 + hh_][0:64, :].rearrange("p (h t) -> p h t", t=128)), [psf[4 + hh_]], [QrT])
            if STG < 1.6:
                dma("sp", x1_d[i * 128:(i + 1) * 128, :], xti[:], reads=[xti], writes=[DR["x1"]])
                continue
            for j in range(4):
                op("pe", lambda e, j=j: e.matmul(psf[2][0:64, j * 128:(j + 1) * 128], lhsT=qb[:, 1, 512 + j * 64:512 + (j + 1) * 64], rhs=identb[:], start=True, stop=True), [qb, identb], [psf[2]])
            if STG < 1.8:
                dma("sp", x1_d[i * 128:(i + 1) * 128, :], xti[:], reads=[xti], writes=[DR["x1"]])
                continue
            slot = i % KWR
            for g in range(2):
                if "ksel" not in SKIP:
                    op("act", lambda e, g=g: e.copy(out=kselT[:, g, :], in_=psf[2][0:64, g * 128:(g + 1) * 128]), [psf[2]], [kselT])
                if "kwin" not in SKIP:
                    op("dve", lambda e, g=g: e.tensor_copy(out=kwinR[:, g, slot * 128:(slot + 1) * 128], in_=psf[2][0:64, (2 + g) * 128:(3 + g) * 128]), [psf[2]], [kwinR])
                if "vsel" not in SKIP:
                    op("dve", lambda e, g=g: e.tensor_copy(out=vsel[:, 0, g, 0:64], in_=tok[:, TVS + g * 64:TVS + (g + 1) * 64]), [tok], [vsel])
                if "vwin" not in SKIP:
                    op("dve", lambda e, g=g: e.tensor_copy(out=vwinR[:, slot, g, 0:64], in_=tok[:, TVW + g * 64:TVW + (g + 1) * 64]), [tok], [vwinR])
            dma("sp", kselD[i], kselT[:].rearrange("p g t -> p (g t)"), reads=[kselT], writes=[DR["kselD"]])
            dma("sp", vselD[i], vsel[:].rearrange("p o g d -> p (o g d)"), reads=[vsel], writes=[DR["vselD"]])
            if "gates" not in SKIP:
                op("act", lambda e: e.activation(out=gates[:], in_=tok[:, TGL:TGL + 24], func=AF.Sigmoid), [tok], [gates])

            if STG < 3:
                dma("sp", x1_d[i * 128:(i + 1) * 128, :], xti[:], reads=[xti], writes=[DR["x1"]])
                continue
            m0 = 0
            nb = 8
            nbase = 8 * i
            for kv in range(2):
                for g in range(2):
                    for hc in range(2):
                        bk = psf[2 + (kv * 4 + g * 2 + hc) % 4]
                        for j in range(16):
                            op("pe", lambda e, kv=kv, g=g, hc=hc, j=j, bk=bk: e.matmul(bk[:, 0:nb], lhsT=w1b[kv][:, j, hc * 128:(hc + 1) * 128], rhs=stk[kv].ap(g * 144 + 4 * (j // 2) + (j % 2), [[16, nb]]), start=(j == 0), stop=(j == 15)), [w1b[kv], stk[kv]], [bk])
                        op("act", lambda e, kv=kv, g=g, hc=hc, bk=bk: e.activation(out=hidT.ap(((kv * 2 + g) * 2 + hc) * 8, [[1, nb]]), in_=bk[:, 0:nb], func=AF.Gelu_apprx_tanh, bias=cbias[:, kv * 2 + hc:kv * 2 + hc + 1]), [bk, cbias], [hidT])
            for kv in range(2):
                for g in range(2):
                    bk = psf[2 + kv * 2 + g]
                    for hc in range(2):
                        op("pe", lambda e, kv=kv, g=g, hc=hc, bk=bk: e.matmul(bk[0:64, 0:nb], lhsT=w2b[kv][:, hc, :], rhs=hidT.ap(((kv * 2 + g) * 2 + hc) * 8, [[1, nb]]), start=(hc == 0), stop=(hc == 1)), [w2b[kv], hidT], [bk])
                    dstT = kcT if kv == 0 else vcT
                    op("dve", lambda e, g=g, bk=bk, dstT=dstT: e.tensor_copy(out=dstT[:, g, nbase:nbase + nb], in_=bk[0:64, 0:nb]), [bk], [dstT])
            for kv in range(2):
                op("pool", lambda e, kv=kv: e.tensor_copy(out=stk[kv][0:64, :, 0:16], in_=stk[kv][0:64, :, 128:144]), [stk[kv]], [stk[kv]])
                op("pool", lambda e, kv=kv: e.tensor_copy(out=stk[kv][64:128, :, 0:14], in_=stk[kv][64:128, :, 128:142]), [stk[kv]], [stk[kv]])
            ctc = min((8 * i + 7) // 128, NCT - 1)
            for g in range(2):
                op("pe", lambda e, g=g: e.matmul(psf[3][:, g * 64:(g + 1) * 64], lhsT=vcT[:, g, ctc * 128:(ctc + 1) * 128], rhs=identb[0:64, 0:64], start=True, stop=True), [vcT, identb], [psf[3]])
                op("dve", lambda e, g=g: e.tensor_copy(out=vcx[:, ctc, g, 0:64], in_=psf[3][:, g * 64:(g + 1) * 64]), [psf[3]], [vcx])
                if ctc == 0:
                    op("dve", lambda e, g=g: e.memset(vcx[0:1, 0, g, 0:64], 0.0), [], [vcx])

            if STG < 4:
                dma("sp", x1_d[i * 128:(i + 1) * 128, :], xti[:], reads=[xti], writes=[DR["x1"]])
                continue
            si = 0
            for g in range(2):
                QTg = QT.ap(g * 512, [[1, 512]], npart=64)
                QrTg = QrT.ap(g * 512, [[1, 512]], npart=64)
                accc = lambda r: psf[2 + r // 2][:, (r % 2) * VW:(r % 2 + 1) * VW]
                for ct in range(ctc + 1):
                    dl = i - 16 * ct
                    masks = []
                    if dl < 16:
                        masks.append((identb[:], ("perhead", cmaskb[:, dl * 128:(dl + 1) * 128]), [identb, cmaskb]))
                    attn_tile(kcT[:, g, ct * 128:(ct + 1) * 128], [kcT], QTg, masks, vcx[:, ct, g, 0:VW], [vcx], accc, [psf[2], psf[3]], ct == 0, ct == ctc, si, starts=(0, 2))
                    si += 1
                for r in range(4):
                    op("dve", lambda e, r=r: e.tensor_scalar(out=st8[:, 8 + r:9 + r], in0=psf[2 + r // 2][:, (r % 2) * VW + VW - 1:(r % 2) * VW + VW], scalar1=1e-30, scalar2=None, op0=ALU.max), [psf[2], psf[3]], [st8])
                op("dve", lambda e: e.reciprocal(out=st8[:, 8:12], in_=st8[:, 8:12]), [st8], [st8])
                op("dve", lambda e: e.tensor_scalar(out=impb[:, 0, :], in0=psf[2][:, 64:64 + NSEL], scalar1=st8[:, 8:9], scalar2=None, op0=ALU.mult), [psf[2], st8], [impb])
                for r in range(1, 4):
                    op("dve", lambda e, r=r: e.scalar_tensor_tensor(out=impb[:, 0, :], in0=psf[2 + r // 2][:, (r % 2) * VW + 64:(r % 2) * VW + 64 + NSEL], scalar=st8[:, 8 + r:9 + r], in1=impb[:, 0, :], op0=ALU.mult, op1=ALU.add), [psf[2], psf[3], st8, impb], [impb])
                op("dve", lambda e, g=g: e.tensor_tensor(out=coef[:, 0, g * 4:(g + 1) * 4], in0=st8[:, 8:12], in1=gates.ap(g * 12 + 0, [[3, 4]]), op=ALU.mult), [st8, gates], [coef])
                for r in range(4):
                    op("dve", lambda e, r=r, g=g: e.tensor_scalar(out=onsa[:, (g * 4 + r) * 64:(g * 4 + r + 1) * 64], in0=psf[2 + r // 2][:, (r % 2) * VW:(r % 2) * VW + 64], scalar1=coef[:, 0, g * 4 + r:g * 4 + r + 1], scalar2=None, op0=ALU.mult), [psf[2], psf[3], coef], [onsa])
                op("dve", lambda e: e.tensor_scalar(out=impb[:, 1, :], in0=cf("jb"), scalar1=float(2 * i - 1), scalar2=None, op0=ALU.is_ge), [cst], [impb])
                op("dve", lambda e: e.tensor_tensor(out=impb[:, 1, :], in0=impb[:, 1, :], in1=cf("col0"), op=ALU.max), [impb, cst], [impb])
                op("dve", lambda e: e.scalar_tensor_tensor(out=impb[:, 2, :], in0=impb[:, 1, :], scalar=1e4, in1=impb[:, 0, :], op0=ALU.mult, op1=ALU.add), [impb], [impb])
                op("dve", lambda e: e.tensor_scalar(out=impb[:, 3, :], in0=cf("jb"), scalar1=float(2 * i), scalar2=-1e30, op0=ALU.is_gt, op1=ALU.mult), [cst], [impb])
                op("dve", lambda e: e.tensor_tensor(out=impb[:, 2, :], in0=impb[:, 2, :], in1=impb[:, 3, :], op=ALU.add), [impb], [impb])
                op("dve", lambda e: e.max(out=m8[:, 0:8], in_=impb[:, 2, :]), [impb], [m8])
                op("dve", lambda e: e.match_replace(out=impb[:, 4, :], in_to_replace=m8[:, 0:8], in_values=impb[:, 2, :], imm_value=-3e38), [impb, m8], [impb])
                op("dve", lambda e: e.max(out=m8[:, 8:16], in_=impb[:, 4, :]), [impb], [m8])
                op("dve", lambda e: e.memset(nselb[:], 0.0), [], [nselb])
                op("dve", lambda e: e.tensor_scalar(out=nselb[:, 0:NSEL], in0=impb[:, 2, :], scalar1=m8[:, 15:16], scalar2=NEG, op0=ALU.is_lt, op1=ALU.mult), [impb, m8], [nselb])
                op("pe", lambda e: e.transpose(out=psb[1][:, 128:256], in_=nselb[:], identity=identb[:]), [nselb, identb], [psb[1]])
                op("act", lambda e, g=g: e.copy(out=nselT[:, g, :].rearrange("p (r t) -> p r t", t=128), in_=psb[1].ap(128, [[0, 4], [1, 128]])), [psb[1]], [nselT])
                accs = lambda r: psf[4][:, r * 65:(r + 1) * 65]
                for kt in range(i + 1):
                    ek = Ekt[si % 4]
                    op("pool", lambda e, ek=ek, kt=kt: e.tensor_copy(out=ek[:].rearrange("p (s k) -> p s k", k=64), in_=identb.ap(2 * kt, [[1, 2], [0, 64]])), [identb], [ek])
                    masks = [(ek[:], nselT[:, g, :], [ek, nselT])]
                    if kt == i:
                        masks.append((identb[:], caus4[:], [identb, caus4]))
                    if kt == i:
                        attn_tile(kselT[:, g, :], [kselT], QrTg, masks, vsel[:, 0, g, 0:65], [vsel], accs, [psf[4]], kt == 0, kt == i, si)
                    else:
                        kr_ = kring[si % 4]
                        vr_ = vring[si % 4]
                        dma("sp", kr_[:], kselD[kt, :, g * 128:(g + 1) * 128], reads=[DR["kselD"]], writes=[kr_])
                        dma("sp", vr_[:], vselD[kt, :, g * 66:(g + 1) * 66], reads=[DR["vselD"]], writes=[vr_])
                        attn_tile(kr_[:], [kr_], QrTg, masks, vr_[:, 0:65], [vr_], accs, [psf[4]], kt == 0, kt == i, si)
                    si += 1
                accw = lambda r: psf[5][:, r * 65:(r + 1) * 65]
                k0 = max(0, i - 4)
                for kt in range(k0, i + 1):
                    masks = []
                    if kt == i:
                        masks.append((identb[:], caus4[:], [identb, caus4]))
                    elif kt == i - 4:
                        masks.append((identb[:], upper4[:], [identb, upper4]))
                    sl = kt % KWR
                    attn_tile(kwinR[:, g, sl * 128:(sl + 1) * 128], [kwinR], QrTg, masks, vwinR[:, sl, g, 0:65], [vwinR], accw, [psf[5]], kt == k0, kt == i, si)
                    si += 1
                for bi, bk in ((1, psf[4]), (2, psf[5])):
                    op("dve", lambda e, bk=bk: e.tensor_scalar(out=st8[:, 12:16], in0=bk.ap(64, [[65, 4]]), scalar1=1e-30, scalar2=None, op0=ALU.max), [bk], [st8])
                    op("dve", lambda e: e.reciprocal(out=st8[:, 12:16], in_=st8[:, 12:16]), [st8], [st8])
                    op("dve", lambda e, bi=bi, g=g: e.tensor_tensor(out=coef[:, bi, g * 4:(g + 1) * 4], in0=st8[:, 12:16], in1=gates.ap(g * 12 + bi, [[3, 4]]), op=ALU.mult), [st8, gates], [coef])
                    op("dve", lambda e, bk=bk, bi=bi, g=g: e.tensor_tensor(out=otmp[:].rearrange("p (r d) -> p r d", d=64), in0=bk.ap(0, [[65, 4], [1, 64]]), in1=coef.ap(bi * 8 + g * 4, [[1, 4], [0, 64]]), op=ALU.mult), [bk, coef], [otmp])
                    op("dve", lambda e, g=g: e.tensor_tensor(out=onsa[:, g * 256:(g + 1) * 256], in0=onsa[:, g * 256:(g + 1) * 256], in1=otmp[:], op=ALU.add), [onsa, otmp], [onsa])
            rms_rstd(onsa[:], 512, st8[:, 1:2], [onsa])
            op("dve", lambda e: e.scalar_tensor_tensor(out=ob[:, 0:512], in0=onsa[:], scalar=st8[:, 1:2], in1=NSAW, op0=ALU.mult, op1=ALU.mult), [onsa, st8, prm], [ob])

            if STG < 5:
                dma("sp", x1_d[i * 128:(i + 1) * 128, :], xti[:], reads=[xti], writes=[DR["x1"]])
                continue
            for k in range(4):
                wv = cw.ap(k, [[4, 8], [0, 128]])
                if k == 0:
                    op("dve", lambda e, wv=wv: e.tensor_tensor(out=cacc[:], in0=xbcp[:, :, 0:128], in1=wv, op=ALU.mult), [xbcp, cw], [cacc])
                else:
                    op("pool", lambda e, wv=wv, k=k: e.tensor_tensor(out=ctmp[:], in0=xbcp[:, :, k:k + 128], in1=wv, op=ALU.mult), [xbcp, cw], [ctmp])
                    op("dve", lambda e: e.tensor_tensor(out=cacc[:], in0=cacc[:], in1=ctmp[:], op=ALU.add), [cacc, ctmp], [cacc])
            op("dve", lambda e: e.tensor_tensor(out=cacc[:], in0=cacc[:], in1=cbs.ap(0, [[1, 8], [0, 128]]), op=ALU.add), [cacc, cbs], [cacc])
            op("act", lambda e: e.activation(out=xbcs[:], in_=cacc[:], func=AF.Silu), [cacc], [xbcs])
            op("pool", lambda e: e.tensor_copy(out=xbcp[:, :, 0:3], in_=xbcp[:, :, 128:131]), [xbcp], [xbcp])
            for c in range(6):
                op("pe", lambda e, c=c: e.transpose(out=psb[0][:, c * 128:(c + 1) * 128], in_=xbcs[:, c, :], identity=identb[:]), [xbcs, identb], [psb[0]])
            op("act", lambda e: e.copy(out=xstok[:], in_=psb[0][:, 0:512]), [psb[0]], [xstok])
            op("dve", lambda e: e.tensor_copy(out=Btok[:], in_=psb[0][:, 512:768].rearrange("p (g n) -> p g n", n=128)), [psb[0]], [Btok])
            op("dve", lambda e: e.tensor_tensor(out=dts[:, 0:8], in0=tok[:, TDT:TDT + 8], in1=DTB, op=ALU.add), [tok, prm], [dts])
            op("act", lambda e: e.activation(out=dts[:, 0:8], in_=dts[:, 0:8], func=AF.Exp), [dts], [dts])
            op("act", lambda e: e.activation(out=dts[:, 0:8], in_=dts[:, 0:8], func=AF.Ln, bias=1.0), [dts], [dts])
            op("dve", lambda e: e.tensor_tensor(out=dts[:, 8:16], in0=dts[:, 0:8], in1=AN, op=ALU.mult), [dts, prm], [dts])
            op("dve", lambda e: e.tensor_tensor(out=Xd[:].rearrange("p (h d) -> p h d", d=64), in0=xstok[:].rearrange("p (h d) -> p h d", d=64), in1=dts.ap(0, [[1, 8], [0, 64]]), op=ALU.mult), [xstok, dts], [Xd])
            op("dve", lambda e: e.tensor_tensor(out=atri[:], in0=dts.ap(8, [[1, 8], [0, 128]]), in1=cst.ap(cfo("tri"), [[0, 8], [1, 128]]), op=ALU.mult), [dts, cst], [atri])
            op("pe", lambda e: e.matmul(psf[2][:, 0:8], lhsT=cf("m1"), rhs=dts[:, 8:16], start=True, stop=True), [cst, dts], [psf[2]])
            op("act", lambda e: e.activation(out=dts[:, 16:24], in_=psf[2][:, 0:8], func=AF.Exp), [psf[2]], [dts])
            op("dve", lambda e: e.tensor_tensor(out=Xdd[:].rearrange("p (h d) -> p h d", d=64), in0=Xd[:].rearrange("p (h d) -> p h d", d=64), in1=dts.ap(16, [[1, 8], [0, 64]]), op=ALU.mult), [Xd, dts], [Xdd])
            for g in range(2):
                op("pe", lambda e, g=g: e.matmul(psf[3][:, g * 128:(g + 1) * 128], lhsT=xbcs[:, 4 + g, :], rhs=xbcs[:, 6 + g, :], start=True, stop=True), [xbcs], [psf[3]])
            op("dve", lambda e: e.tensor_tensor(out=cbm[:], in0=psf[3][:, 0:256].rearrange("p (g l) -> p g l", l=128), in1=cst.ap(cfo("tri"), [[0, 2], [1, 128]]), op=ALU.mult), [psf[3], cst], [cbm])
            for g in range(2):
                at_g = atri.ap(g * 512, [[1, 512]])
                op("pe", lambda e, at_g=at_g: e.matmul(psf[4][:, :], lhsT=cf("m1"), rhs=at_g, start=True, stop=True), [cst, atri], [psf[4]])
                op("pe", lambda e, at_g=at_g: e.matmul(psf[5][:, :], lhsT=cf("ones"), rhs=at_g, start=True, stop=True), [cst, atri], [psf[5]])
                op("act", lambda e: e.activation(out=lexp[:], in_=psf[4][:, :].rearrange("p (h l) -> p h l", l=128), func=AF.Exp), [psf[4]], [lexp])
                op("act", lambda e, g=g: e.activation(out=eab[:, g * 4:(g + 1) * 4, :], in_=psf[5][:, :].rearrange("p (h l) -> p h l", l=128), func=AF.Exp), [psf[5]], [eab])
                op("dve", lambda e, g=g: e.tensor_tensor(out=cbl[:, g * 4:(g + 1) * 4, :], in0=lexp[:], in1=cbm.ap(g * 128, [[0, 4], [1, 128]]), op=ALU.mult), [lexp, cbm], [cbl])
                op("pool", lambda e, g=g: e.tensor_tensor(out=cth[:, g * 4:(g + 1) * 4, :], in0=eab[:, g * 4:(g + 1) * 4, :], in1=xbcs.ap((6 + g) * 128, [[0, 4], [1, 128]]), op=ALU.mult), [eab, xbcs], [cth])
            for h in range(8):
                op("pe", lambda e, h=h: e.matmul(psf[2][:, h * 64:(h + 1) * 64], lhsT=cbl[:, h, :], rhs=Xd[:, h * 64:(h + 1) * 64], start=True, stop=False), [cbl, Xd], [psf[2]])
                op("pe", lambda e, h=h: e.matmul(psf[2][:, h * 64:(h + 1) * 64], lhsT=cth[:, h, :], rhs=hstb[:, h * 64:(h + 1) * 64], start=False, stop=True), [cth, hstb], [psf[2]])
            for g in range(2):
                op("pe", lambda e, g=g: e.matmul(psf[3][:, g * 256:(g + 1) * 256], lhsT=Btok[:, g, :], rhs=Xdd[:, g * 256:(g + 1) * 256], start=True, stop=True), [Btok, Xdd], [psf[3]])
            op("dve", lambda e: e.tensor_tensor(out=hst[:].rearrange("p (h d) -> p h d", d=64), in0=hst[:].rearrange("p (h d) -> p h d", d=64), in1=eab.ap(127, [[128, 8], [0, 64]]), op=ALU.mult), [hst, eab], [hst])
            op("dve", lambda e: e.tensor_tensor(out=hst[:], in0=hst[:], in1=psf[3][:, :], op=ALU.add), [hst, psf[3]], [hst])
            op("pool", lambda e: e.tensor_copy(out=hstb[:], in_=hst[:]), [hst], [hstb])
            op("dve", lambda e: e.tensor_tensor(out=yss[:], in0=xstok[:], in1=DSK, op=ALU.mult), [xstok, prm], [yss])
            op("dve", lambda e: e.tensor_tensor(out=yss[:], in0=yss[:], in1=psf[2][:, :], op=ALU.add), [yss, psf[2]], [yss])
            op("act", lambda e: e.activation(out=zs[:], in_=tok[:, TZ:TZ + 512], func=AF.Silu), [tok], [zs])
            op("dve", lambda e: e.tensor_tensor(out=yss[:], in0=yss[:], in1=zs[:], op=ALU.mult), [yss, zs], [yss])
            rms_rstd(yss[:], 512, st8[:, 2:3], [yss])
            op("dve", lambda e: e.scalar_tensor_tensor(out=ob[:, 512:1024], in0=yss[:], scalar=st8[:, 2:3], in1=SSDW, op0=ALU.mult, op1=ALU.mult), [yss, st8, prm], [ob])

            if STG < 6:
                dma("sp", x1_d[i * 128:(i + 1) * 128, :], xti[:], reads=[xti], writes=[DR["x1"]])
                continue
            to_featT(ob, oT, None, None, 8, [ob])
            for hh in range(2):
                for c in range(8):
                    op("pe", lambda e, c=c, hh=hh: e.matmul(psf[2 + hh][:, :], lhsT=oT[:, c, :], rhs=woutb[:, c, hh * 512:(hh + 1) * 512], start=(c == 0), stop=(c == 7)), [oT, woutb], [psf[2 + hh]])
            for hh in range(2):
                op("dve", lambda e, hh=hh: e.tensor_tensor(out=junk[:, hh * 512:(hh + 1) * 512], in0=psf[2 + hh][:, :], in1=gb[:, 0, hh * 512:(hh + 1) * 512], op=ALU.mult), [psf[2 + hh], gb], [junk])
            op("dve", lambda e: e.tensor_tensor(out=xti[:], in0=xti[:], in1=junk[:], op=ALU.add), [xti, junk], [xti])
            dma("sp", x1_d[i * 128:(i + 1) * 128, :], xti[:], reads=[xti], writes=[DR["x1"]])
            if dbg:
                dma("sp", dbg_d[i, :, 0:512], onsa[:], reads=[onsa], writes=[DR["dbg"]])
                dma("sp", dbg_d[i, :, 512:1024], yss[:], reads=[yss], writes=[DR["dbg"]])
                dma("sp", dbg_d[i, :, 1024:1024 + NSEL], impb[:, 2, :], reads=[impb], writes=[DR["dbg"]])
                dma("sp", dbg_d[i, :, 1536:2048], tok[:, TZ:TZ + 512], reads=[tok], writes=[DR["dbg"]])
                dma("sp", dbg_d[i, :, 1280:1288], dts[:, 0:8], reads=[dts], writes=[DR["dbg"]])
                dma("sp", dbg_d[i, :, 1288:1312], gates[:, :], reads=[gates], writes=[DR["dbg"]])

        kb.finish([DR["x1"], DR["dbg"]])
        kb.barrier()
        es_mix.close()
        es_mixp.close()
        kb.es = es
        build.kb = kb
        if PEERON:
            NTP = NT if NTILES is None else NTILES
            wqb = kb.sb("wqb", [128, 8, 2048], BF16)
            keysTb = kb.sb("keysTb", [128, 16, 128], BF16)
            fnwb = kb.sb("fnwb", [128, D], F32)
            LR = kb.sb("LR", [128, 16384], BF16)
            LT = kb.sb("LT", [128, 128, 128], BF16)
            RT = kb.sb("RT", [128, 128, 128], BF16)
            scb = kb.sb("scb", [128, 2048], F32)
            cand = kb.sb("cand", [128, 2048], F32)
            qnb = kb.sb("qnb", [128, 2048], BF16)
            qnT = kb.sb("qnT", [128, 16, 128], BF16)
            h2T = kb.sb("h2T", [128, 8, 128], BF16)
            pxt = kb.sb("pxt", [128, D], F32)
            pjk = kb.sb("pjk", [128, D], F32)
            pxn = kb.sb("pxn", [128, D], BF16)
            stp = kb.sb("stp", [128, 16, 16], F32)
            ctop = kb.sb("ctop", [128, 8, 16], F32)
            mrp = kb.sb("mrp", [128, 256], F32)
            sm = kb.sb("sm", [128, 512], F32)
            bh = kb.sb("bh", [128, 8, 128], F32)
            dnbuf = [kb.sb("dnbuf%d" % i_, [128, 8, 128], BF16) for i_ in range(2)]
            upbuf = [kb.sb("upbuf%d" % i_, [128, D], BF16) for i_ in range(2)]
            Gs = kb.sb("Gs", [128, 128], F32)
            GW = [kb.sb("GW%d" % i_, [128, 128], BF16) for i_ in range(2)]
            cstg = [scb, cand]
            cstb = [qnb, kb.sb("cstb1", [128, 1024], BF16)]
            dma("sp", fnwb[:], fnw_d.partition_broadcast(128), writes=[fnwb])
            qnws = kb.sb("qnws", [128, 2], F32)
            dma("sp", qnws[:], qnw_d[:, :], writes=[qnws])
            for kc in range(8):
                s_ = cstg[kc % 2]
                dma("sp", s_[:], wq_d[kc * 128:(kc + 1) * 128, :], writes=[s_])
                op("dve", lambda e, s_=s_, kc=kc: e.tensor_copy(out=wqb[:, kc, :], in_=s_[:]), [s_], [wqb])
            s_ = cstg[0]
            dma("sp", s_[:], keys_d.rearrange("p a n -> p (a n)"), writes=[s_])
            for hk in range(16):
                op("dve", lambda e, hk=hk, s_=s_: e.tensor_scalar(out=keysTb[:, hk, :], in0=s_[:, hk * 128:(hk + 1) * 128], scalar1=qnws[:, hk % 2:hk % 2 + 1], scalar2=None, op0=ALU.mult), [s_, qnws], [keysTb])
            for i2 in range(128):
                for which in range(2):
                    s_ = cstg[(i2 * 2 + which) % 2]
                    b_ = cstb[(i2 * 2 + which) % 2]
                    if which == 0:
                        dma("sp", s_[:, 0:1024], down_d[i2].rearrange("p c i -> p (c i)"), writes=[s_])
                    else:
                        dma("sp", s_[:, 0:1024], up_d[i2], writes=[s_])
                    op("dve" if which == 0 else "pool", lambda e, s_=s_, b_=b_: e.tensor_copy(out=b_[:, 0:1024], in_=s_[:, 0:1024]), [s_], [b_])
                    dma("sp", (dnb_d if which == 0 else upb_d)[i2], b_[:, 0:1024], reads=[b_], writes=[DR["dnb" if which == 0 else "upb"]])

            def top16(src_ap, dst_ap, n, src_bufs, dst_tt):
                op("dve", lambda e: e.max(out=dst_ap(0), in_=src_ap), src_bufs, [dst_tt])
                op("dve", lambda e: e.match_replace(out=mrp[:, 0:n], in_to_replace=dst_ap(0), in_values=src_ap, imm_value=-3e38), src_bufs + [dst_tt], [mrp])
                op("dve", lambda e: e.max(out=dst_ap(8), in_=mrp[:, 0:n]), [mrp], [dst_tt])

            for i in range(NTP):
                dma("sp", pxt[:], x1_d[i * 128:(i + 1) * 128, :], reads=[DR["x1"]], writes=[pxt])
                op("act", lambda e: e.activation(out=pjk[:], in_=pxt[:], func=AF.Square, accum_out=sm[:, 0:1]), [pxt], [pjk, sm])
                op("dve", lambda e: e.tensor_scalar(out=sm[:, 0:1], in0=sm[:, 0:1], scalar1=1.0 / D, scalar2=EPS, op0=ALU.mult, op1=ALU.add), [sm], [sm])
                op("act", lambda e: e.activation(out=sm[:, 0:1], in_=sm[:, 0:1], func=AF.Sqrt), [sm], [sm])
                op("dve", lambda e: e.reciprocal(out=sm[:, 0:1], in_=sm[:, 0:1]), [sm], [sm])
                op("dve", lambda e: e.tensor_scalar(out=pxn[:], in0=pxt[:], scalar1=sm[:, 0:1], scalar2=None, op0=ALU.mult), [pxt, sm], [pxn])
                for c in range(8):
                    op("pe", lambda e, c=c: e.transpose(out=psb[0][:, c * 128:(c + 1) * 128], in_=pxn[:, c * 128:(c + 1) * 128], identity=identb[:]), [pxn, identb], [psb[0]])
                for c in range(8):
                    op("act", lambda e, c=c: e.activation(out=h2T[:, c, :], in_=psb[0][:, c * 128:(c + 1) * 128], func=AF.Identity, scale=msc[:, 8 + c:9 + c], bias=modfm[:, 24 + c:25 + c]), [psb[0], msc, modfm], [h2T])
                for bq in range(4):
                    for kc in range(8):
                        op("pe", lambda e, bq=bq, kc=kc: e.matmul(psf[2 + bq][:, :], lhsT=h2T[:, kc, :], rhs=wqb[:, kc, bq * 512:(bq + 1) * 512], start=(kc == 0), stop=(kc == 7)), [h2T, wqb], [psf[2 + bq]])
                for h in range(8):
                    op("act", lambda e, h=h: e.activation(out=pjk[:, 0:256], in_=psf[2 + h // 2][:, (h % 2) * 256:(h % 2 + 1) * 256], func=AF.Square, accum_out=sm[:, 8 + h:9 + h]), [psf[2 + h // 2]], [pjk, sm])
                op("dve", lambda e: e.tensor_scalar(out=sm[:, 8:16], in0=sm[:, 8:16], scalar1=1.0 / 256, scalar2=EPS, op0=ALU.mult, op1=ALU.add), [sm], [sm])
                op("act", lambda e: e.activation(out=sm[:, 8:16], in_=sm[:, 8:16], func=AF.Sqrt), [sm], [sm])
                op("dve", lambda e: e.reciprocal(out=sm[:, 8:16], in_=sm[:, 8:16]), [sm], [sm])
                for bq in range(4):
                    op("dve", lambda e, bq=bq: e.tensor_tensor(out=qnb[:, bq * 512:(bq + 1) * 512].rearrange("p (h d) -> p h d", d=256), in0=psf[2 + bq][:, :].rearrange("p (h d) -> p h d", d=256), in1=sm.ap(8 + 2 * bq, [[1, 2], [0, 256]]), op=ALU.mult), [psf[2 + bq], sm], [qnb])
                for hk in range(16):
                    op("pe", lambda e, hk=hk: e.transpose(out=psb[hk // 8][:, (hk % 8) * 128:(hk % 8 + 1) * 128], in_=qnb[:, hk * 128:(hk + 1) * 128], identity=identb[:]), [qnb, identb], [psb[hk // 8]])
                for bb in range(2):
                    op("act" if bb == 0 else "dve", (lambda e, bb=bb: e.copy(out=qnT[:, bb * 8:(bb + 1) * 8, :], in_=psb[bb][:, :].rearrange("p (c t) -> p c t", t=128))) if bb == 0 else (lambda e, bb=bb: e.tensor_copy(out=qnT[:, bb * 8:(bb + 1) * 8, :], in_=psb[bb][:, :].rearrange("p (c t) -> p c t", t=128))), [psb[bb]], [qnT])
                for hk in range(16):
                    op("pe", lambda e, hk=hk: e.matmul(psf[2 + hk // 4][:, (hk % 4) * 128:(hk % 4 + 1) * 128], lhsT=qnT[:, hk, :], rhs=keysTb[:, hk, :], start=True, stop=True), [qnT, keysTb], [psf[2 + hk // 4]])
                for bq in range(4):
                    op("act" if bq % 2 else "dve", (lambda e, bq=bq: e.copy(out=scb[:, bq * 512:(bq + 1) * 512], in_=psf[2 + bq][:, :])) if bq % 2 else (lambda e, bq=bq: e.tensor_copy(out=scb[:, bq * 512:(bq + 1) * 512], in_=psf[2 + bq][:, :])), [psf[2 + bq]], [scb])
                for hk in range(16):
                    top16(scb[:, hk * 128:(hk + 1) * 128], lambda o, hk=hk: stp[:, hk, o:o + 8], 128, [scb], stp)
                op("dve", lambda e: e.tensor_tensor(out=cand[:].rearrange("p (h a b) -> p h a b", a=16, b=16), in0=stp.ap(0, [[32, 8], [1, 16], [0, 16]]), in1=stp.ap(16, [[32, 8], [0, 16], [1, 16]]), op=ALU.add), [stp], [cand])
                for h in range(8):
                    top16(cand[:, h * 256:(h + 1) * 256], lambda o, h=h: ctop[:, h, o:o + 8], 256, [cand], ctop)
                op("dve", lambda e: e.tensor_tensor(out=sm[:, 128:256].rearrange("p (h a) -> p h a", a=16), in0=ctop[:, :, :], in1=ctop.ap(0, [[16, 8], [0, 16]]), op=ALU.subtract), [ctop], [sm])
                op("act", lambda e: e.activation(out=sm[:, 128:256], in_=sm[:, 128:256], func=AF.Exp), [sm], [sm])
                op("dve", lambda e: e.tensor_reduce(out=sm[:, 16:24], in_=sm[:, 128:256].rearrange("p (h a) -> p h a", a=16), axis=AX.X, op=ALU.add), [sm], [sm])
                op("dve", lambda e: e.reciprocal(out=sm[:, 16:24], in_=sm[:, 16:24]), [sm], [sm])
                op("dve", lambda e: e.tensor_tensor(out=sm[:, 256:384].rearrange("p (h a) -> p h a", a=16), in0=stp.ap(0, [[32, 8], [1, 16]]), in1=stp.ap(0, [[32, 8], [0, 16]]), op=ALU.subtract), [stp], [sm])
                op("act", lambda e: e.activation(out=sm[:, 256:384], in_=sm[:, 256:384], func=AF.Exp), [sm], [sm])
                op("dve", lambda e: e.tensor_tensor(out=sm[:, 384:512].rearrange("p (h a) -> p h a", a=16), in0=ctop.ap(15, [[16, 8], [0, 16]]), in1=stp.ap(0, [[32, 8], [1, 16]]), op=ALU.subtract), [ctop, stp], [sm])
                op("dve", lambda e: e.tensor_tensor(out=bh[:], in0=scb.ap(128, [[256, 8], [1, 128]]), in1=stp.ap(16, [[32, 8], [0, 128]]), op=ALU.subtract), [scb, stp], [bh])
                op("act", lambda e: e.activation(out=bh[:], in_=bh[:], func=AF.Exp), [bh], [bh])
                op("dve", lambda e: e.tensor_tensor(out=bh[:], in0=bh[:], in1=sm.ap(16, [[1, 8], [0, 128]]), op=ALU.mult), [bh, sm], [bh])
                LRv = LR[:].rearrange("p (h a n) -> p h a n", a=16, n=128)
                for side in range(2):
                    if side == 0:
                        op("dve", lambda e: e.tensor_tensor(out=LRv, in0=scb.ap(0, [[256, 8], [0, 16], [1, 128]]), in1=stp.ap(0, [[32, 8], [1, 16], [0, 128]]), op=ALU.is_equal), [scb, stp], [LR])
                        op("pool", lambda e: e.tensor_tensor(out=LRv, in0=LRv, in1=sm.ap(256, [[16, 8], [1, 16], [0, 128]]), op=ALU.mult), [LR, sm], [LR])
                    else:
                        op("dve", lambda e: e.tensor_tensor(out=LRv, in0=scb.ap(128, [[256, 8], [0, 16], [1, 128]]), in1=sm.ap(384, [[16, 8], [1, 16], [0, 128]]), op=ALU.is_ge), [scb, sm], [LR])
                        op("pool", lambda e: e.tensor_tensor(out=LRv, in0=LRv, in1=bh.ap(0, [[128, 8], [0, 16], [1, 128]]), op=ALU.mult), [LR, bh], [LR])
                    dstT = LT if side == 0 else RT
                    for ii in range(128):
                        op("pe", lambda e, ii=ii: e.transpose(out=psb[(ii // 8) % 2][:, (ii % 8) * 128:(ii % 8 + 1) * 128], in_=LR.ap(ii, [[128, 128]]), identity=identb[:]), [LR, identb], [psb[(ii // 8) % 2]])
                        if ii % 8 == 7:
                            i0 = ii - 7
                            eng = "act" if (ii // 8) % 2 else "dve"
                            op(eng, (lambda e, i0=i0, ii=ii, dstT=dstT: e.copy(out=dstT[:, i0:i0 + 8, :], in_=psb[(ii // 8) % 2][:, :].rearrange("p (c t) -> p c t", t=128))) if eng == "act" else (lambda e, i0=i0, ii=ii, dstT=dstT: e.tensor_copy(out=dstT[:, i0:i0 + 8, :], in_=psb[(ii // 8) % 2][:, :].rearrange("p (c t) -> p c t", t=128))), [psb[(ii // 8) % 2]], [dstT])
                for t in range(128):
                    bk = psf[2 + (t // 4) % 2]
                    op("pe", lambda e, t=t, bk=bk: e.matmul(bk[:, (t % 4) * 128:(t % 4 + 1) * 128], lhsT=LT.ap(t, [[128, 128]]), rhs=RT.ap(t, [[128, 128]]), start=True, stop=True), [LT, RT], [bk])
                    if t % 4 == 3:
                        t0 = t - 3
                        eng = "act" if (t // 4) % 2 else "dve"
                        op(eng, (lambda e, t0=t0, bk=bk: e.copy(out=LR[:, t0 * 128:(t0 + 4) * 128], in_=bk[:, :])) if eng == "act" else (lambda e, t0=t0, bk=bk: e.tensor_copy(out=LR[:, t0 * 128:(t0 + 4) * 128], in_=bk[:, :])), [bk], [LR])
                for i2 in range(128):
                    dn_ = dnbuf[i2 % 2]
                    up_ = upbuf[i2 % 2]
                    dma("sp", dn_[:].rearrange("p c i -> p (c i)"), dnb_d[i2], reads=[DR["dnb"]], writes=[dn_])
                    dma("pool", up_[:], upb_d[i2], reads=[DR["upb"]], writes=[up_])
                    bk = psf[2 + i2 % 2]
                    for kc in range(8):
                        op("pe", lambda e, kc=kc, dn_=dn_, bk=bk: e.matmul(bk[:, 0:128], lhsT=dn_[:, kc, :], rhs=h2T[:, kc, :], start=(kc == 0), stop=(kc == 7)), [dn_, h2T], [bk])
                    op("act", lambda e, bk=bk: e.activation(out=Gs[:], in_=bk[:, 0:128], func=AF.Gelu_apprx_tanh), [bk], [Gs])
                    gw_ = GW[i2 % 2]
                    op("dve", lambda e, gw_=gw_, i2=i2: e.tensor_tensor(out=gw_[:], in0=Gs[:], in1=LR.ap(i2, [[128, 128]]), op=ALU.mult), [Gs, LR], [gw_])
                    for hh in range(2):
                        op("pe", lambda e, hh=hh, gw_=gw_, up_=up_, i2=i2: e.matmul(psf[4 + hh][:, :], lhsT=gw_[:], rhs=up_[:, hh * 512:(hh + 1) * 512], start=(i2 == 0), stop=(i2 == 127)), [gw_, up_], [psf[4 + hh]])
                for hh in range(2):
                    op("dve", lambda e, hh=hh: e.tensor_tensor(out=pjk[:, hh * 512:(hh + 1) * 512], in0=psf[4 + hh][:, :], in1=gb[:, 1, hh * 512:(hh + 1) * 512], op=ALU.mult), [psf[4 + hh], gb], [pjk])
                op("dve", lambda e: e.tensor_tensor(out=pxt[:], in0=pxt[:], in1=pjk[:], op=ALU.add), [pxt, pjk], [pxt])
                op("act", lambda e: e.activation(out=pjk[:], in_=pxt[:], func=AF.Square, accum_out=sm[:, 1:2]), [pxt], [pjk, sm])
                op("dve", lambda e: e.tensor_scalar(out=sm[:, 1:2], in0=sm[:, 1:2], scalar1=1.0 / D, scalar2=EPS, op0=ALU.mult, op1=ALU.add), [sm], [sm])
                op("act", lambda e: e.activation(out=sm[:, 1:2], in_=sm[:, 1:2], func=AF.Sqrt), [sm], [sm])
                op("dve", lambda e: e.reciprocal(out=sm[:, 1:2], in_=sm[:, 1:2]), [sm], [sm])
                op("dve", lambda e: e.scalar_tensor_tensor(out=pjk[:], in0=pxt[:], scalar=sm[:, 1:2], in1=fnwb[:], op0=ALU.mult, op1=ALU.mult), [pxt, sm, fnwb], [pjk])
                dma("sp", out_d[i * 128:(i + 1) * 128, :], pjk[:], reads=[pjk], writes=[DR["out"]])
            kb.finish([DR["out"]])
    return nc, cvals


def prep_core_inputs(inp, b, S, cvals):
    f = lambda a: np.ascontiguousarray(a, dtype=np.float32)
    fm = lambda v: f(np.asarray(v).reshape(-1, 128).T)
    m = {}
    m["x"] = f(inp["x"][b, :S])
    m["cT"] = fm(inp["c"][b])
    m["w_ada"] = f(inp["w_ada"][0])
    m["b_ada"] = f(inp["b_ada"][0])
    m["norm1_w"] = fm(inp["norm1_w"][0])
    m["norm2_w"] = fm(inp["norm2_w"][0])
    m["final_norm_w"] = f(inp["final_norm_w"])
    m["w_in"] = f(inp["w_in"][0])
    lidx = np.array([[4 * (j // 2) + (j % 2) + 2 * hf for hf in range(2)] for j in range(16)])
    rows = (lidx[:, :, None] * 64 + np.arange(64)[None, None, :]).reshape(-1)
    for nm in ("k", "v"):
        m["cmp_pe_" + nm] = fm(np.asarray(inp["cmp_pe_" + nm][0]).reshape(-1)[rows])
        m["cmp_w1_" + nm] = f(np.asarray(inp["cmp_w1_" + nm][0])[rows])
    m["cmp_w2_k"] = f(inp["cmp_w2_k"][0])
    m["cmp_w2_v"] = f(inp["cmp_w2_v"][0])
    m["nsa_norm_w"] = f(inp["nsa_norm_w"][0])
    cwt = np.asarray(inp["conv_w"][0])
    m["conv_w"] = f(cwt.reshape(4, 8, 128).transpose(2, 1, 0))
    m["conv_b"] = fm(inp["conv_b"][0])
    m["dt_bias"] = f(inp["dt_bias"][0])
    m["a_log"] = f(inp["a_log"][0])
    m["d_skip"] = f(np.repeat(np.asarray(inp["d_skip"][0]), 64))
    m["ssd_norm_w"] = f(inp["ssd_norm_w"][0])
    m["w_out"] = f(inp["w_out"][0])
    m["peer_wq"] = f(inp["peer_wq"][0])
    m["peer_qnorm_w"] = fm(inp["peer_qnorm_w"][0])
    sk = np.asarray(inp["peer_sub_keys"][0])
    m["peer_keysT"] = f(sk.reshape(16, 128, 128).transpose(2, 0, 1))
    dn = np.asarray(inp["peer_down"][0]).reshape(128, 128, 8, 128)
    m["peer_downT"] = f(dn.transpose(1, 3, 2, 0))
    upp = np.asarray(inp["peer_up"][0]).reshape(128, 128, D)
    m["peer_up"] = f(upp.transpose(1, 0, 2))
    m["consts"] = cvals
    return m


def kernel(**inputs):
    S = inputs["x"].shape[1]
    B = inputs["x"].shape[0]
    inp = {k: np.asarray(v) for k, v in inputs.items()}
    nc, cvals = build(S)
    in_maps = [prep_core_inputs(inp, b, S, cvals) for b in range(B)]
    res = run_bass_kernel_spmd(nc, in_maps, core_ids=list(range(B)))
    return np.stack([r["out"] for r in res.results], axis=0).astype(np.float32)
```

```python
import numpy as np
import concourse.bass as bass
import concourse.mybir as mybir
from concourse.bass_utils import run_bass_kernel_spmd
from contextlib import ExitStack

F32 = mybir.dt.float32
BF16 = mybir.dt.bfloat16
ALU = mybir.AluOpType
AF = mybir.ActivationFunctionType
AX = mybir.AxisListType

D = 1024
NEG = -30000.0
EPS = 1e-6


class Buf:
    __slots__ = ("name", "lw", "rd", "ps")

    def __init__(self, name=""):
        self.name = name
        self.lw = None
        self.rd = []
        self.ps = False


class TT:
    def __init__(self, h, shape, name):
        self.h = h
        self.shape = list(shape)
        self.free = int(np.prod(shape[1:]))
        self.b = Buf(name)

    def __getitem__(self, k):
        return self.h[k]

    def ap(self, off, dims, p0=0, npart=128):
        return bass.AP(self.h, p0 * self.free + off, [[self.free, npart]] + [list(d) for d in dims])


class KB:
    def __init__(self, nc, es):
        self.nc = nc
        self.es = es
        self.engs = {"pe": nc.tensor, "act": nc.scalar, "dve": nc.vector, "pool": nc.gpsimd, "sp": nc.sync}
        self.sems = {}
        self.cnt = {}
        for e in self.engs:
            self.sems[e] = es.enter_context(nc.semaphore("s_" + e))
            self.cnt[e] = 0
        self.seen = {e: {} for e in self.engs}
        self.dpool = {}
        self.dnext = {}
        for q in ("sp", "pool", "act"):
            self.dpool[q] = []
            for i in range(16 if q == "sp" else 2):
                k = "d_%s%d" % (q, i)
                self.sems[k] = es.enter_context(nc.semaphore(k))
                self.cnt[k] = 0
                self.dpool[q].append(k)
            self.dnext[q] = 0
        self.ninst = 0

    def sb(self, name, shape, dt):
        return TT(self.es.enter_context(self.nc.sbuf_tensor(name, list(shape), dt)), shape, name)

    def ps(self, name, shape, dt):
        t = TT(self.es.enter_context(self.nc.psum_tensor(name, list(shape), dt)), shape, name)
        t.b.ps = True
        return t

    def _need(self, e, deps):
        need = {}
        for d in deps:
            if d is None:
                continue
            k, v, _ = d
            if self.seen[e].get(k, 0) >= v:
                continue
            if need.get(k, 0) < v:
                need[k] = v
        for k, v in need.items():
            self.engs[e].wait_ge(self.sems[k], v)
            self.seen[e][k] = v

    def _deps(self, e, reads, writes):
        deps = []
        for b in reads:
            if b.lw is not None:
                deps.append(b.lw)
            if b.ps:
                for r in b.rd:
                    if r[2] != e:
                        deps.append(r)
        for b in writes:
            if b.lw is not None and not (b.lw[0] == e and b.lw[2] == e):
                deps.append(b.lw)
            for r in b.rd:
                if r[2] != e or r[0] != e:
                    deps.append(r)
        if e == "pe":
            deps = [d for d in deps if d[0] != "pe"]
        return deps

    def op(self, e, fn, reads=(), writes=()):
        reads = [getattr(r, "b", r) for r in reads]
        writes = [getattr(w, "b", w) for w in writes]
        self._need(e, self._deps(e, reads, writes))
        inst = fn(self.engs[e])
        self.cnt[e] += 1
        inst.then_inc(self.sems[e], 1)
        tok = (e, self.cnt[e], e)
        for b in reads:
            b.rd.append(tok)
            if len(b.rd) > 32:
                best = {}
                for k, v, en in b.rd:
                    if k not in best or best[k][1] < v:
                        best[k] = (k, v, en)
                b.rd = list(best.values())
        for b in writes:
            b.lw = tok
            b.rd = []
        self.ninst += 1
        return inst

    def dma(self, q, out, in_, reads=(), writes=(), **kw):
        reads = [getattr(r, "b", r) for r in reads]
        writes = [getattr(w, "b", w) for w in writes]
        self._need(q, self._deps(q, reads, writes))
        k = self.dpool[q][self.dnext[q] % len(self.dpool[q])]
        self.dnext[q] += 1
        if self.cnt[k] > 0:
            self._need(q, [(k, self.cnt[k], q)])
        inst = self.engs[q].dma_start(out=out, in_=in_, **kw)
        self.cnt[k] += 16
        inst.then_inc(self.sems[k], 16)
        tok = (k, self.cnt[k], q)
        for b in reads:
            b.rd.append(tok)
        for b in writes:
            b.lw = tok
            b.rd = []
        self.ninst += 1
        return tok

    def barrier(self):
        for e in self.engs:
            deps = [(k, v, "x") for k, v in self.cnt.items() if v > 0 and k != e]
            self._need(e, deps)

    def finish(self, bufs):
        deps = []
        for b in bufs:
            b = getattr(b, "b", b)
            if b.lw is not None:
                deps.append(b.lw)
        self._need("sp", deps)


def make_consts(S):
    NT = S // 128
    NSEL = S // 64
    p = np.arange(128)
    c = {}
    c["ident"] = np.eye(128, dtype=np.float32)
    c["tri"] = (p[:, None] <= p[None, :]).astype(np.float32)
    c["m1"] = (p[:, None] > p[None, :]).astype(np.float32)
    c["ones"] = np.ones((128, 128), np.float32)
    c["caus"] = np.where(p[:, None] <= p[None, :], 0.0, NEG).astype(np.float32)
    c["upper"] = np.where(p[:, None] > p[None, :], 0.0, NEG).astype(np.float32)
    cm = np.zeros((128, 17, 128), np.float32)
    for dl in range(17):
        cm[:, dl, :] = np.where(16 * p[:, None] - p[None, :] <= 128 * dl - 15, 0.0, NEG)
    c["cmask"] = cm.reshape(128, 17 * 128)
    NCT = max(1, S // 2048)
    n_cmp = S // 16 - 1
    ov = np.zeros((128, NCT, NSEL), np.float32)
    for n in range(n_cmp):
        for j in range(NSEL):
            o = min(16 * n + 32, 64 * j + 64) - max(16 * n, 64 * j)
            if o > 0:
                ov[(n + 1) % 128, (n + 1) // 128, j] = o / 32.0
    c["ov"] = ov.reshape(128, NCT * NSEL)
    c["jb"] = (np.arange(NSEL)[None, :] - (p[:, None] // 64)).astype(np.float32)
    col0 = np.zeros((128, NSEL), np.float32)
    col0[:, 0] = 1.0
    c["col0"] = col0
    half = 8
    inv = (500000.0 ** (-np.arange(half, dtype=np.float32) / half)).astype(np.float32)
    pos = np.arange(S, dtype=np.float32)
    ang = (pos[:, None] * inv[None, :]).astype(np.float32)
    rope = np.concatenate([np.cos(ang), np.sin(ang)], axis=1).astype(np.float32)
    c["rope"] = np.ascontiguousarray(rope.reshape(NT, 128, 16).transpose(1, 0, 2)).reshape(128, NT * 16)
    names = ["tri", "m1", "ones", "jb", "col0", "rope", "ident", "caus", "upper", "cmask", "ov"]
    offs = {}
    o = 0
    for n in names:
        offs[n] = (o, c[n].shape[1])
        o += c[n].shape[1]
    return np.concatenate([c[n] for n in names], axis=1).astype(np.float32), offs


C_Q, C_KC, C_VC, C_KS, C_VS, C_KW, C_VW, C_GL, C_Z, C_XBC, C_DT = 0, 512, 640, 768, 896, 1024, 1152, 1280, 1304, 1816, 2840


def build(S, dbg=False, STG=99, NTILES=None, SKIP=(), PEERON=True):
    NT = S // 128
    NSEL = S // 64
    NCT = max(1, S // 2048)
    TP = 256
    NP2 = S // TP
    cvals, coffs = make_consts(S)
    NCF = cvals.shape[1]
    nc = bass.Bass("TRN2", target_bir_lowering=False)
    dI = lambda n, s, dt=F32: nc.dram_tensor(n, list(s), dt, kind="ExternalInput").ap()
    x_d = dI("x", [S, D])
    cT_d = dI("cT", [128, 8])
    wada_d = dI("w_ada", [D, 6 * D])
    bada_d = dI("b_ada", [6 * D])
    n1_d = dI("norm1_w", [128, 8])
    n2_d = dI("norm2_w", [128, 8])
    fnw_d = dI("final_norm_w", [D])
    win_d = dI("w_in", [D, 2848])
    pek_d = dI("cmp_pe_k", [128, 16])
    pev_d = dI("cmp_pe_v", [128, 16])
    w1k_d = dI("cmp_w1_k", [2048, 256])
    w1v_d = dI("cmp_w1_v", [2048, 256])
    w2k_d = dI("cmp_w2_k", [256, 64])
    w2v_d = dI("cmp_w2_v", [256, 64])
    nsaw_d = dI("nsa_norm_w", [512])
    convw_d = dI("conv_w", [128, 8, 4])
    convb_d = dI("conv_b", [128, 8])
    dtb_d = dI("dt_bias", [8])
    alog_d = dI("a_log", [8])
    dsk_d = dI("d_skip", [512])
    ssdw_d = dI("ssd_norm_w", [512])
    wout_d = dI("w_out", [D, D])
    wq_d = dI("peer_wq", [D, 2048] if PEERON else [1, 1])
    qnw_d = dI("peer_qnorm_w", [128, 2])
    keys_d = dI("peer_keysT", [128, 16, 128] if PEERON else [1, 1, 1])
    down_d = dI("peer_downT", [128, 128, 8, 128] if PEERON else [1, 1, 1, 1])
    up_d = dI("peer_up", [128, 128, D] if PEERON else [1, 1, 1])
    const_d = dI("consts", [128, NCF])
    out_d = nc.dram_tensor("out", [S, D], F32, kind="ExternalOutput").ap()
    skind = "ExternalOutput" if dbg else "Internal"
    x1_d = nc.dram_tensor("x1s", [S, D], F32, kind=skind).ap()
    kselD = nc.dram_tensor("kselD", [NT, 64, 256], BF16, kind="Internal").ap()
    vselD = nc.dram_tensor("vselD", [NT, 128, 132], BF16, kind="Internal").ap()
    h2T_d = nc.dram_tensor("h2Ts", [NT, 128, 8 * 128], BF16, kind="Internal").ap()
    wg_d = nc.dram_tensor("wgs", [NT, 128, 128 * 128], BF16, kind="Internal").ap()
    dnb_d = nc.dram_tensor("dnbs", [128, 128, 8 * 128], BF16, kind="Internal").ap()
    upb_d = nc.dram_tensor("upbs", [128, 128, D], BF16, kind="Internal").ap()
    if dbg:
        dbg_d = nc.dram_tensor("dbg", [NT, 128, 2048], F32, kind="ExternalOutput").ap()

    with ExitStack() as es:
        kb = KB(nc, es)
        op, dma = kb.op, kb.dma
        DR = {n: Buf(n) for n in ("x1", "h2T", "wg", "dnb", "upb", "out", "dbg", "kselD", "vselD")}
        psf = [kb.ps("psf%d" % i, [128, 512], F32) for i in range(6)]
        psb = [kb.ps("psb%d" % i, [128, 1024], BF16) for i in range(2)]
        RES = ("tri", "m1", "ones", "jb", "col0", "rope")
        r_lo = coffs["tri"][0]
        r_hi = coffs["rope"][0] + coffs["rope"][1]
        NRES = r_hi - r_lo
        cst = kb.sb("cst", [128, NRES], F32)

        def cfo(name):
            return coffs[name][0] - r_lo

        def cf(name, lo=0, n=None):
            o, w = coffs[name]
            n = w - lo if n is None else n
            return cst[:, o - r_lo + lo:o - r_lo + lo + n]

        identb = kb.sb("identb", [128, 128], BF16)
        modfm = kb.sb("modfm", [128, 48], F32)
        gb = kb.sb("gb", [128, 2, D], F32)
        msc = kb.sb("msc", [128, 16], F32)
        es_mixp = ExitStack()
        kb.es = es_mixp
        cmaskb = kb.sb("cmaskb", [128, 17 * 128], BF16)
        Ekt = [kb.sb("Ekt%d" % i_, [128, 128], BF16) for i_ in range(4)]
        caus4 = kb.sb("caus4", [128, 512], BF16)
        upper4 = kb.sb("upper4", [128, 512], BF16)
        winb = kb.sb("winb", [128, 8, 2848], BF16)
        woutb = kb.sb("woutb", [128, 8, D], BF16)
        w1b = [kb.sb("w1b%d" % i, [128, 16, 256], BF16) for i in range(2)]
        w2b = [kb.sb("w2b%d" % i, [128, 2, 64], BF16) for i in range(2)]
        wcmp = kb.sb("wcmp", [128, 8, 4, 128], BF16)
        cbias = kb.sb("cbias", [128, 4], F32)
        prm = kb.sb("prm", [128, 1536 + 32], F32)
        cw = kb.sb("cw", [128, 8, 4], F32)
        cbs = kb.sb("cbs", [128, 8], F32)
        KWR = 5
        kselT = kb.sb("kselT", [64, 2, 128], BF16)
        kring = [kb.sb("kring%d" % i_, [64, 128], BF16) for i_ in range(4)]
        vring = [kb.sb("vring%d" % i_, [128, 66], BF16) for i_ in range(4)]
        kwinR = kb.sb("kwinR", [64, 2, KWR * 128], BF16)
        vsel = kb.sb("vsel", [128, 1, 2, 66], BF16)
        vwinR = kb.sb("vwinR", [128, KWR, 2, 66], BF16)
        kcT = kb.sb("kcT", [64, 2, NCT * 128], BF16)
        vcT = kb.sb("vcT", [64, 2, NCT * 128], BF16)
        VW = 64 + NSEL + 1
        vcx = kb.sb("vcx", [128, NCT, 2, VW + 1], BF16)
        stk = [kb.sb("stk%d" % i, [128, 2, 144], BF16) for i in range(2)]
        xbcp = kb.sb("xbcp", [128, 8, 131], F32)
        hst = kb.sb("hst", [128, 512], F32)
        hstb = kb.sb("hstb", [128, 512], BF16)

        es_setup = ExitStack()
        kb.es = es_setup
        dma("sp", cst[:], const_d[:, r_lo:r_hi], writes=[cst])
        stg = [kb.sb("stg%d" % i, [128, 3072], F32) for i in range(2)]
        stgi = [0]

        def load_cast(dst_tt, dst_ap_fn, src_ap_fn, nchunks, width, eng="pool"):
            for ci in range(nchunks):
                s_ = stg[stgi[0] % 2]
                stgi[0] += 1
                dma("sp", s_[:, 0:width], src_ap_fn(ci), writes=[s_])
                op(eng, lambda e, s_=s_, ci=ci: e.tensor_copy(out=dst_ap_fn(ci), in_=s_[:, 0:width]), [s_], [dst_tt])

        def cload(name):
            o, w = coffs[name]
            return const_d[:, o:o + w]
        load_cast(identb, lambda ci: identb[:], lambda ci: cload("ident"), 1, 128, "dve")
        load_cast(cmaskb, lambda ci: cmaskb[:], lambda ci: cload("cmask"), 1, 17 * 128, "dve")
        for nm_, dst_ in (("caus", caus4), ("upper", upper4)):
            s_ = stg[stgi[0] % 2]
            stgi[0] += 1
            dma("sp", s_[:, 0:128], cload(nm_), writes=[s_])
            op("dve", lambda e, s_=s_, dst_=dst_: e.tensor_copy(out=dst_[:].rearrange("p (r t) -> p r t", t=128), in_=s_.ap(0, [[0, 4], [1, 128]])), [s_], [dst_])
        cTs = kb.sb("cTs", [128, 8], F32)
        dma("sp", cTs[:], cT_d[:, :], writes=[cTs])
        crep = kb.sb("crep", [128, 8, 128], F32)
        for kc in range(8):
            op("dve", lambda e, kc=kc: e.tensor_copy(out=crep[:, kc, :], in_=cTs[:, kc:kc + 1].to_broadcast([128, 128])), [cTs], [crep])
        badafm = kb.sb("badafm", [128, 48], F32)
        dma("sp", badafm[:], bada_d.rearrange("(c p) -> p c", p=128), writes=[badafm], allow_slow_non_contiguous=True)
        badab = kb.sb("badab", [128, 2, D], F32)
        dma("sp", badab[:, 0, :], bada_d[2 * D:3 * D].partition_broadcast(128), writes=[badab])
        dma("sp", badab[:, 1, :], bada_d[5 * D:6 * D].partition_broadcast(128), writes=[badab])
        for part in range(2):
            for kc in range(8):
                w_ = stg[stgi[0] % 2]
                stgi[0] += 1
                dma("sp", w_[:, 0:3072], wada_d[kc * 128:(kc + 1) * 128, part * 3072:(part + 1) * 3072], writes=[w_])
                for ol in range(24):
                    oc = part * 24 + ol
                    op("pe", lambda e, w_=w_, oc=oc, ol=ol, kc=kc: e.matmul(psf[0][:, oc:oc + 1], lhsT=w_[:, ol * 128:(ol + 1) * 128], rhs=cTs[:, kc:kc + 1], start=(kc == 0 and ol == 0), stop=(kc == 7)), [w_, cTs], [psf[0]])
                gi = part
                for hh in range(2):
                    op("pe", lambda e, w_=w_, gi=gi, hh=hh, kc=kc: e.matmul(psf[1 + gi * 2 + hh][:, :], lhsT=crep[:, kc, :], rhs=w_[:, 2048 + hh * 512:2048 + hh * 512 + 512], start=(kc == 0), stop=(kc == 7)), [w_, crep], [psf[1 + gi * 2 + hh]])
        op("dve", lambda e: e.tensor_tensor(out=modfm[:], in0=psf[0][:, 0:48], in1=badafm[:], op=ALU.add), [psf[0], badafm], [modfm])
        for gi in range(2):
            for hh in range(2):
                op("dve", lambda e, gi=gi, hh=hh: e.tensor_tensor(out=gb[:, gi, hh * 512:(hh + 1) * 512], in0=psf[1 + gi * 2 + hh][:, :], in1=badab[:, gi, hh * 512:(hh + 1) * 512], op=ALU.add), [psf[1 + gi * 2 + hh], badab], [gb])
        nws = kb.sb("nws", [128, 16], F32)
        dma("sp", nws[:, 0:8], n1_d[:, :], writes=[nws])
        dma("sp", nws[:, 8:16], n2_d[:, :], writes=[nws])
        op("dve", lambda e: e.scalar_tensor_tensor(out=msc[:, 0:8], in0=modfm[:, 8:16], scalar=1.0, in1=nws[:, 0:8], op0=ALU.add, op1=ALU.mult), [modfm, nws], [msc])
        op("dve", lambda e: e.scalar_tensor_tensor(out=msc[:, 8:16], in0=modfm[:, 32:40], scalar=1.0, in1=nws[:, 8:16], op0=ALU.add, op1=ALU.mult), [modfm, nws], [msc])
        load_cast(winb, lambda ci: winb[:, ci, :], lambda ci: win_d[ci * 128:(ci + 1) * 128, :], 8, 2848)
        load_cast(woutb, lambda ci: woutb[:, ci, :], lambda ci: wout_d[ci * 128:(ci + 1) * 128, :], 8, D)
        for i_, wd in enumerate((w1k_d, w1v_d)):
            wv_ = wd.rearrange("(j p) h -> p j h", p=128)
            for hf in range(2):
                load_cast(w1b[i_], lambda ci, i_=i_, hf=hf: w1b[i_][:, hf * 8:(hf + 1) * 8, :], lambda ci, wv_=wv_, hf=hf: wv_[:, hf * 8:(hf + 1) * 8, :], 1, 2048)
        for i_, wd in enumerate((w2k_d, w2v_d)):
            load_cast(w2b[i_], lambda ci, i_=i_: w2b[i_][:, :, :], lambda ci, wd=wd: wd.rearrange("(c p) d -> p c d", p=128), 1, 128)
        for kv, c0 in enumerate((C_KC, C_VC)):
            for g in range(2):
                for hf in range(2):
                    op("pool", lambda e, kv=kv, g=g, hf=hf, c0=c0: e.tensor_copy(out=wcmp[:, :, kv * 2 + g, hf * 64:(hf + 1) * 64], in_=winb[:, :, c0 + g * 64:c0 + (g + 1) * 64]), [winb], [wcmp])
        pes = kb.sb("pes", [128, 2, 16], F32)
        dma("sp", pes[:, 0, :], pek_d[:, :], writes=[pes])
        dma("sp", pes[:, 1, :], pev_d[:, :], writes=[pes])
        peb = kb.sb("peb", [128, 2, 16], BF16)
        op("dve", lambda e: e.tensor_copy(out=peb[:], in_=pes[:]), [pes], [peb])
        for kv in range(2):
            for hc in range(2):
                for j in range(16):
                    op("pe", lambda e, kv=kv, hc=hc, j=j: e.matmul(psf[5][:, kv * 2 + hc:kv * 2 + hc + 1], lhsT=w1b[kv][:, j, hc * 128:(hc + 1) * 128], rhs=peb[:, kv, j:j + 1], start=(j == 0), stop=(j == 15)), [w1b[kv], peb], [psf[5]])
        op("dve", lambda e: e.tensor_copy(out=cbias[:], in_=psf[5][:, 0:4]), [psf[5]], [cbias])
        dma("sp", prm[:, 0:512], nsaw_d.partition_broadcast(128), writes=[prm])
        dma("sp", prm[:, 512:1024], ssdw_d.partition_broadcast(128), writes=[prm])
        dma("sp", prm[:, 1024:1536], dsk_d.partition_broadcast(128), writes=[prm])
        dma("sp", prm[:, 1536:1544], dtb_d.partition_broadcast(128), writes=[prm])
        dma("sp", prm[:, 1544:1552], alog_d.partition_broadcast(128), writes=[prm])
        op("act", lambda e: e.activation(out=prm[:, 1552:1560], in_=prm[:, 1544:1552], func=AF.Exp), [prm], [prm])
        op("dve", lambda e: e.tensor_scalar(out=prm[:, 1552:1560], in0=prm[:, 1552:1560], scalar1=-1.0, scalar2=None, op0=ALU.mult), [prm], [prm])
        NSAW, SSDW, DSK, DTB, AN = prm[:, 0:512], prm[:, 512:1024], prm[:, 1024:1536], prm[:, 1536:1544], prm[:, 1552:1560]
        dma("sp", cw[:], convw_d[:, :, :], writes=[cw])
        dma("sp", cbs[:], convb_d[:, :], writes=[cbs])
        for t_ in (kcT, vcT, vcx, vsel, vwinR, stk[0], stk[1], xbcp, hst, hstb):
            op("pool", lambda e, t_=t_: e.memset(t_[:], 0.0), [], [t_])
        for ct in range(NCT):
            s_ = stg[stgi[0] % 2]
            stgi[0] += 1
            o_ov = coffs["ov"][0]
            dma("sp", s_[:, 0:NSEL], const_d[:, o_ov + ct * NSEL:o_ov + (ct + 1) * NSEL], writes=[s_])
            for g in range(2):
                op("pool", lambda e, ct=ct, g=g, s_=s_: e.tensor_copy(out=vcx[:, ct, g, 64:64 + NSEL], in_=s_[:, 0:NSEL]), [s_], [vcx])
                op("pool", lambda e, ct=ct, g=g: e.memset(vcx[:, ct, g, 64 + NSEL:VW + 1], 1.0), [], [vcx])
        op("pool", lambda e: e.memset(vcx[0:1, 0, :, :], 0.0), [], [vcx])
        for g in range(2):
            op("pool", lambda e, g=g: e.memset(vsel[:, :, g, 64:66], 1.0), [], [vsel])
            op("pool", lambda e, g=g: e.memset(vwinR[:, :, g, 64:66], 1.0), [], [vwinR])
        kb.barrier()
        es_setup.close()
        es_mix = ExitStack()
        kb.es = es_mix
        xt = [kb.sb("xt%d" % i, [128, D], F32) for i in range(2)]
        junk = kb.sb("junk", [128, D], F32)
        st8 = kb.sb("st8", [128, 64], F32)
        xnb = kb.sb("xnb", [128, D], BF16)
        hT = kb.sb("hT", [128, 8, 128], BF16)
        tok = kb.sb("tok", [128, 1568], F32)
        TQ, TKS, TKW, TVS, TVW, TGL, TZ, TDT = 0, 512, 640, 768, 896, 1024, 1048, 1560
        qb = kb.sb("qb", [128, 2, 768], BF16)
        QT = kb.sb("QT", [64, 8, 128], BF16)
        QrT = kb.sb("QrT", [64, 8, 128], BF16)
        rtmp = kb.sb("rtmp", [128, 4, 12, 8], F32)
        gates = kb.sb("gates", [128, 24], F32)
        pT = [kb.sb("pT%d" % i, [128, 512], BF16) for i in range(2)]
        nselT = kb.sb("nselT", [128, 2, 512], BF16)
        impb = kb.sb("impb", [128, 6, NSEL], F32)
        nselb = kb.sb("nselb", [128, 128], BF16)
        m8 = kb.sb("m8", [128, 16], F32)
        coef = kb.sb("coef", [128, 3, 8], F32)
        onsa = kb.sb("onsa", [128, 512], F32)
        otmp = kb.sb("otmp", [128, 256], F32)
        ob = kb.sb("ob", [128, D], BF16)
        oT = kb.sb("oT", [128, 8, 128], BF16)
        hidT = kb.sb("hidT", [128, 64], BF16)
        xbcs = kb.sb("xbcs", [128, 8, 128], BF16)
        cacc = kb.sb("cacc", [128, 8, 128], F32)
        ctmp = kb.sb("ctmp", [128, 8, 128], F32)
        xstok = kb.sb("xstok", [128, 512], F32)
        Xd = kb.sb("Xd", [128, 512], BF16)
        Xdd = kb.sb("Xdd", [128, 512], BF16)
        Btok = kb.sb("Btok", [128, 2, 128], BF16)
        dts = kb.sb("dts", [128, 32], F32)
        atri = cacc
        cbm = kb.sb("cbm", [128, 2, 128], F32)
        lexp = kb.sb("lexp", [128, 4, 128], F32)
        cbl = kb.sb("cbl", [128, 8, 128], BF16)
        eab = ctmp
        cth = kb.sb("cth", [128, 8, 128], BF16)
        yss = kb.sb("yss", [128, 512], F32)
        zs = kb.sb("zs", [128, 512], F32)

        def rms_rstd(src_ap, n, dst_ap, src_bufs):
            op("act", lambda e: e.activation(out=junk[:, 0:n], in_=src_ap, func=AF.Square, accum_out=dst_ap), src_bufs, [junk, st8])
            op("dve", lambda e: e.tensor_scalar(out=dst_ap, in0=dst_ap, scalar1=1.0 / n, scalar2=EPS, op0=ALU.mult, op1=ALU.add), [st8], [st8])
            op("act", lambda e: e.activation(out=dst_ap, in_=dst_ap, func=AF.Sqrt), [st8], [st8])
            op("dve", lambda e: e.reciprocal(out=dst_ap, in_=dst_ap), [st8], [st8])

        def to_featT(src_bf, dstT, sc_ap_fn, sh_ap_fn, nch, src_bufs):
            for c in range(nch):
                op("pe", lambda e, c=c: e.transpose(out=psb[c // 8 % 2][:, (c % 8) * 128:(c % 8 + 1) * 128], in_=src_bf[:, c * 128:(c + 1) * 128], identity=identb[:]), src_bufs + [identb], [psb[c // 8 % 2]])
                if c % 8 == 7 or c == nch - 1:
                    c0 = c - c % 8
                    if sc_ap_fn is None:
                        op("act", lambda e, c0=c0, c=c: e.copy(out=dstT[:, c0:c + 1, :], in_=psb[c // 8 % 2][:, 0:(c - c0 + 1) * 128].rearrange("p (c t) -> p c t", t=128)), [psb[c // 8 % 2]], [dstT])
                    else:
                        for cc in range(c0, c + 1):
                            op("act", lambda e, cc=cc: e.activation(out=dstT[:, cc, :], in_=psb[cc // 8 % 2][:, (cc % 8) * 128:(cc % 8 + 1) * 128], func=AF.Identity, scale=sc_ap_fn(cc), bias=sh_ap_fn(cc)), [psb[cc // 8 % 2], msc, modfm], [dstT])

        def attn_tile(KT_ap, K_bufs, QTt, masks, V_ap, V_bufs, acc_ps, acc_w, first, last, si, starts=(0,)):
            ps_ = psf[si % 2]
            pt_ = pT[si % 2]
            nm = len(masks)
            op("pe", lambda e: e.matmul(ps_[:, :], lhsT=KT_ap, rhs=QTt, start=True, stop=(nm == 0)), K_bufs + [QT, QrT], [ps_])
            for mi, (ml, mr, mb) in enumerate(masks):
                if isinstance(mr, tuple):
                    for r in range(4):
                        op("pe", lambda e, ml=ml, mr=mr, mi=mi, r=r: e.matmul(ps_[:, r * 128:(r + 1) * 128], lhsT=ml, rhs=mr[1], start=False, stop=(mi == nm - 1)), mb, [ps_])
                else:
                    op("pe", lambda e, ml=ml, mr=mr, mi=mi: e.matmul(ps_[:, :], lhsT=ml, rhs=mr, start=False, stop=(mi == nm - 1)), mb, [ps_])
            op("act", lambda e: e.activation(out=pt_[:], in_=ps_[:, :], func=AF.Exp, scale=0.125), [ps_], [pt_])
            for r in range(4):
                op("pe", lambda e, r=r: e.matmul(acc_ps(r), lhsT=pt_[:, r * 128:(r + 1) * 128], rhs=V_ap, start=(first and r in starts), stop=last), [pt_] + V_bufs, acc_w)

        def rep4(tt, off):
            return tt.ap(off, [[0, 4], [1, 128]])

        for i in range(NT if NTILES is None else NTILES):
            xti = xt[i % 2]
            dma("sp", xti[:], x_d[i * 128:(i + 1) * 128, :], writes=[xti])
            rms_rstd(xti[:], D, st8[:, 0:1], [xti])
            op("dve", lambda e: e.tensor_scalar(out=xnb[:], in0=xti[:], scalar1=st8[:, 0:1], scalar2=None, op0=ALU.mult), [xti, st8], [xnb])
            to_featT(xnb, hT, lambda c: msc[:, c:c + 1], lambda c: modfm[:, c:c + 1], 8, [xnb])
            if STG < 1:
                dma("sp", x1_d[i * 128:(i + 1) * 128, :], xti[:], reads=[xti], writes=[DR["x1"]])
                continue
            if dbg and "dumph" in SKIP:
                op("dve", lambda e: e.tensor_copy(out=junk[:, :], in_=hT[:, :, :].rearrange("p c t -> p (c t)")), [hT], [junk])
                dma("sp", dbg_d[i, :, 0:1024], junk[:, :], reads=[junk], writes=[DR["dbg"]])
                op("dve", lambda e: e.tensor_copy(out=junk[:, :], in_=xnb[:, :]), [xnb], [junk])
                dma("sp", dbg_d[i, :, 1024:2048], junk[:, :], reads=[junk], writes=[DR["dbg"]])
            groups = [(C_Q, 512, TQ, 0), (C_KS, 128, TKS, 1), (C_KW, 128, TKW, 1), (C_VS, 128, TVS, 1), (C_VW, 128, TVW, 1),
                      (C_GL, 24, TGL, 2), (C_Z, 512, TZ, 3), (C_DT, 8, TDT, 2)]
            pso = {0: 0, 1: 0, 2: 0, 3: 0}
            for (c0, w, t0, bank) in groups:
                bk = psf[2 + bank]
                o_ = pso[bank]
                pso[bank] += w
                for kc in range(8):
                    op("pe", lambda e, kc=kc, c0=c0, w=w, o_=o_, bk=bk: e.matmul(bk[:, o_:o_ + w], lhsT=hT[:, kc, :], rhs=winb[:, kc, c0:c0 + w], start=(kc == 0), stop=(kc == 7)), [hT, winb], [bk])
            op("act", lambda e: e.copy(out=tok[:, TQ:TQ + 512], in_=psf[2][:, 0:512]), [psf[2]], [tok])
            op("dve", lambda e: e.tensor_copy(out=tok[:, TKS:TKS + 512], in_=psf[3][:, 0:512]), [psf[3]], [tok])
            op("dve", lambda e: e.tensor_copy(out=tok[:, TGL:TGL + 24], in_=psf[4][:, 0:24]), [psf[4]], [tok])
            op("dve", lambda e: e.tensor_copy(out=tok[:, TDT:TDT + 8], in_=psf[4][:, 24:32]), [psf[4]], [tok])
            op("act", lambda e: e.copy(out=tok[:, TZ:TZ + 512], in_=psf[5][:, 0:512]), [psf[5]], [tok])
            for kv in range(2):
                for g in range(2):
                    idx = kv * 2 + g
                    bk = psf[2 + idx % 2]
                    for kc in range(8):
                        op("pe", lambda e, kc=kc, idx=idx, bk=bk: e.matmul(bk[:, 0:128], lhsT=wcmp[:, kc, idx, :], rhs=hT[:, kc, :], start=(kc == 0), stop=(kc == 7)), [hT, wcmp], [bk])
                    op("act", lambda e, kv=kv, g=g, bk=bk: e.copy(out=stk[kv][0:64, g, 16:144], in_=bk[0:64, 0:128]), [bk], [stk[kv]])
                    op("dve", lambda e, kv=kv, g=g, bk=bk: e.tensor_copy(out=stk[kv][64:128, g, 14:142], in_=bk[64:128, 0:128]), [bk], [stk[kv]])
            for c in range(8):
                bk = psf[4 + c % 2]
                for kc in range(8):
                    op("pe", lambda e, kc=kc, c=c, bk=bk: e.matmul(bk[:, 0:128], lhsT=winb[:, kc, C_XBC + c * 128:C_XBC + (c + 1) * 128], rhs=hT[:, kc, :], start=(kc == 0), stop=(kc == 7)), [hT, winb], [bk])
                op("act" if c % 2 else "dve", (lambda e, c=c, bk=bk: e.copy(out=xbcp[:, c, 3:131], in_=bk[:, 0:128])) if c % 2 else (lambda e, c=c, bk=bk: e.tensor_copy(out=xbcp[:, c, 3:131], in_=bk[:, 0:128])), [bk], [xbcp])

            if STG < 2:
                dma("sp", x1_d[i * 128:(i + 1) * 128, :], xti[:], reads=[xti], writes=[DR["x1"]])
                continue
            op("dve", lambda e: e.tensor_copy(out=qb[:, 0, 0:512], in_=tok[:, TQ:TQ + 512]), [tok], [qb])
            op("pool", lambda e: e.tensor_copy(out=qb[:, 1, :], in_=tok[:, TQ:TQ + 768]), [tok], [qb])
            o_r = coffs["rope"][0] + i * 16
            cosv = cst.ap(cfo("rope") + i * 16, [[0, 12], [1, 8]])
            sinv = cst.ap(cfo("rope") + i * 16 + 8, [[0, 12], [1, 8]])
            x1v = tok.ap(TQ, [[64, 12], [1, 8]])
            x2v = tok.ap(TQ + 8, [[64, 12], [1, 8]])
            op("dve", lambda e: e.tensor_tensor(out=rtmp[:, 0], in0=x1v, in1=cosv, op=ALU.mult), [tok, cst], [rtmp])
            op("dve", lambda e: e.tensor_tensor(out=rtmp[:, 1], in0=x2v, in1=sinv, op=ALU.mult), [tok, cst], [rtmp])
            op("dve", lambda e: e.tensor_tensor(out=rtmp[:, 2], in0=x2v, in1=cosv, op=ALU.mult), [tok, cst], [rtmp])
            op("dve", lambda e: e.tensor_tensor(out=rtmp[:, 3], in0=x1v, in1=sinv, op=ALU.mult), [tok, cst], [rtmp])
            op("dve", lambda e: e.tensor_tensor(out=qb.ap(768, [[64, 12], [1, 8]]), in0=rtmp[:, 0], in1=rtmp[:, 1], op=ALU.subtract), [rtmp], [qb])
            op("dve", lambda e: e.tensor_tensor(out=qb.ap(768 + 8, [[64, 12], [1, 8]]), in0=rtmp[:, 2], in1=rtmp[:, 3], op=ALU.add), [rtmp], [qb])
            if STG < 1.3:
                dma("sp", x1_d[i * 128:(i + 1) * 128, :], xti[:], reads=[xti], writes=[DR["x1"]])
                continue
            for h in range(8):
                op("pe", lambda e, h=h: e.matmul(psf[2 + h // 4][0:64, (h % 4) * 128:(h % 4 + 1) * 128], lhsT=qb[:, 0, h * 64:(h + 1) * 64], rhs=identb[:], start=True, stop=True), [qb, identb], [psf[2 + h // 4]])
                op("pe", lambda e, h=h: e.matmul(psf[4 + h // 4][0:64, (h % 4) * 128:(h % 4 + 1) * 128], lhsT=qb[:, 1, h * 64:(h + 1) * 64], rhs=identb[:], start=True, stop=True), [qb, identb], [psf[4 + h // 4]])
            for hh_ in range(2):
                op("act", lambda e, hh_=hh_: e.copy(out=QT[:, hh_ * 4:(hh_ + 1) * 4, :], in_=psf[2 + hh_][0:64, :].rearrange("p (h t) -> p h t", t=128)), [psf[2 + hh_]], [QT])
                op("dve", lambda e, hh_=hh_: e.tensor_copy(out=QrT[:, hh_ * 4:(hh_ + 1) * 4, :], in_=psf[4 + hh_][0:64, :].rearrange("p (h t) -> p h t", t=128)), [psf[4 + hh_]], [QrT])
            if STG < 1.6:
                dma("sp", x1_d[i * 128:(i + 1) * 128, :], xti[:], reads=[xti], writes=[DR["x1"]])
                continue
            for j in range(4):
                op("pe", lambda e, j=j: e.matmul(psf[2][0:64, j * 128:(j + 1) * 128], lhsT=qb[:, 1, 512 + j * 64:512 + (j + 1) * 64], rhs=identb[:], start=True, stop=True), [qb, identb], [psf[2]])
            if STG < 1.8:
                dma("sp", x1_d[i * 128:(i + 1) * 128, :], xti[:], reads=[xti], writes=[DR["x1"]])
                continue
            slot = i % KWR
            for g in range(2):
                if "ksel" not in SKIP:
                    op("act", lambda e, g=g: e.copy(out=kselT[:, g, :], in_=psf[2][0:64, g * 128:(g + 1) * 128]), [psf[2]], [kselT])
                if "kwin" not in SKIP:
                    op("dve", lambda e, g=g: e.tensor_copy(out=kwinR[:, g, slot * 128:(slot + 1) * 128], in_=psf[2][0:64, (2 + g) * 128:(3 + g) * 128]), [psf[2]], [kwinR])
                if "vsel" not in SKIP:
                    op("dve", lambda e, g=g: e.tensor_copy(out=vsel[:, 0, g, 0:64], in_=tok[:, TVS + g * 64:TVS + (g + 1) * 64]), [tok], [vsel])
                if "vwin" not in SKIP:
                    op("dve", lambda e, g=g: e.tensor_copy(out=vwinR[:, slot, g, 0:64], in_=tok[:, TVW + g * 64:TVW + (g + 1) * 64]), [tok], [vwinR])
            dma("sp", kselD[i], kselT[:].rearrange("p g t -> p (g t)"), reads=[kselT], writes=[DR["kselD"]])
            dma("sp", vselD[i], vsel[:].rearrange("p o g d -> p (o g d)"), reads=[vsel], writes=[DR["vselD"]])
            if "gates" not in SKIP:
                op("act", lambda e: e.activation(out=gates[:], in_=tok[:, TGL:TGL + 24], func=AF.Sigmoid), [tok], [gates])

            if STG < 3:
                dma("sp", x1_d[i * 128:(i + 1) * 128, :], xti[:], reads=[xti], writes=[DR["x1"]])
                continue
            m0 = 0
            nb = 8
            nbase = 8 * i
            for kv in range(2):
                for g in range(2):
                    for hc in range(2):
                        bk = psf[2 + (kv * 4 + g * 2 + hc) % 4]
                        for j in range(16):
                            op("pe", lambda e, kv=kv, g=g, hc=hc, j=j, bk=bk: e.matmul(bk[:, 0:nb], lhsT=w1b[kv][:, j, hc * 128:(hc + 1) * 128], rhs=stk[kv].ap(g * 144 + 4 * (j // 2) + (j % 2), [[16, nb]]), start=(j == 0), stop=(j == 15)), [w1b[kv], stk[kv]], [bk])
                        op("act", lambda e, kv=kv, g=g, hc=hc, bk=bk: e.activation(out=hidT.ap(((kv * 2 + g) * 2 + hc) * 8, [[1, nb]]), in_=bk[:, 0:nb], func=AF.Gelu_apprx_tanh, bias=cbias[:, kv * 2 + hc:kv * 2 + hc + 1]), [bk, cbias], [hidT])
            for kv in range(2):
                for g in range(2):
                    bk = psf[2 + kv * 2 + g]
                    for hc in range(2):
                        op("pe", lambda e, kv=kv, g=g, hc=hc, bk=bk: e.matmul(bk[0:64, 0:nb], lhsT=w2b[kv][:, hc, :], rhs=hidT.ap(((kv * 2 + g) * 2 + hc) * 8, [[1, nb]]), start=(hc == 0), stop=(hc == 1)), [w2b[kv], hidT], [bk])
                    dstT = kcT if kv == 0 else vcT
                    op("dve", lambda e, g=g, bk=bk, dstT=dstT: e.tensor_copy(out=dstT[:, g, nbase:nbase + nb], in_=bk[0:64, 0:nb]), [bk], [dstT])
            for kv in range(2):
                op("pool", lambda e, kv=kv: e.tensor_copy(out=stk[kv][0:64, :, 0:16], in_=stk[kv][0:64, :, 128:144]), [stk[kv]], [stk[kv]])
                op("pool", lambda e, kv=kv: e.tensor_copy(out=stk[kv][64:128, :, 0:14], in_=stk[kv][64:128, :, 128:142]), [stk[kv]], [stk[kv]])
            ctc = min((8 * i + 7) // 128, NCT - 1)
            for g in range(2):
                op("pe", lambda e, g=g: e.matmul(psf[3][:, g * 64:(g + 1) * 64], lhsT=vcT[:, g, ctc * 128:(ctc + 1) * 128], rhs=identb[0:64, 0:64], start=True, stop=True), [vcT, identb], [psf[3]])
                op("dve", lambda e, g=g: e.tensor_copy(out=vcx[:, ctc, g, 0:64], in_=psf[3][:, g * 64:(g + 1) * 64]), [psf[3]], [vcx])
                if ctc == 0:
                    op("dve", lambda e, g=g: e.memset(vcx[0:1, 0, g, 0:64], 0.0), [], [vcx])

            if STG < 4:
                dma("sp", x1_d[i * 128:(i + 1) * 128, :], xti[:], reads=[xti], writes=[DR["x1"]])
                continue
            si = 0
            for g in range(2):
                QTg = QT.ap(g * 512, [[1, 512]], npart=64)
                QrTg = QrT.ap(g * 512, [[1, 512]], npart=64)
                accc = lambda r: psf[2 + r // 2][:, (r % 2) * VW:(r % 2 + 1) * VW]
                for ct in range(ctc + 1):
                    dl = i - 16 * ct
                    masks = []
                    if dl < 16:
                        masks.append((identb[:], ("perhead", cmaskb[:, dl * 128:(dl + 1) * 128]), [identb, cmaskb]))
                    attn_tile(kcT[:, g, ct * 128:(ct + 1) * 128], [kcT], QTg, masks, vcx[:, ct, g, 0:VW], [vcx], accc, [psf[2], psf[3]], ct == 0, ct == ctc, si, starts=(0, 2))
                    si += 1
                for r in range(4):
                    op("dve", lambda e, r=r: e.tensor_scalar(out=st8[:, 8 + r:9 + r], in0=psf[2 + r // 2][:, (r % 2) * VW + VW - 1:(r % 2) * VW + VW], scalar1=1e-30, scalar2=None, op0=ALU.max), [psf[2], psf[3]], [st8])
                op("dve", lambda e: e.reciprocal(out=st8[:, 8:12], in_=st8[:, 8:12]), [st8], [st8])
                op("dve", lambda e: e.tensor_scalar(out=impb[:, 0, :], in0=psf[2][:, 64:64 + NSEL], scalar1=st8[:, 8:9], scalar2=None, op0=ALU.mult), [psf[2], st8], [impb])
                for r in range(1, 4):
                    op("dve", lambda e, r=r: e.scalar_tensor_tensor(out=impb[:, 0, :], in0=psf[2 + r // 2][:, (r % 2) * VW + 64:(r % 2) * VW + 64 + NSEL], scalar=st8[:, 8 + r:9 + r], in1=impb[:, 0, :], op0=ALU.mult, op1=ALU.add), [psf[2], psf[3], st8, impb], [impb])
                op("dve", lambda e, g=g: e.tensor_tensor(out=coef[:, 0, g * 4:(g + 1) * 4], in0=st8[:, 8:12], in1=gates.ap(g * 12 + 0, [[3, 4]]), op=ALU.mult), [st8, gates], [coef])
                for r in range(4):
                    op("dve", lambda e, r=r, g=g: e.tensor_scalar(out=onsa[:, (g * 4 + r) * 64:(g * 4 + r + 1) * 64], in0=psf[2 + r // 2][:, (r % 2) * VW:(r % 2) * VW + 64], scalar1=coef[:, 0, g * 4 + r:g * 4 + r + 1], scalar2=None, op0=ALU.mult), [psf[2], psf[3], coef], [onsa])
                op("dve", lambda e: e.tensor_scalar(out=impb[:, 1, :], in0=cf("jb"), scalar1=float(2 * i - 1), scalar2=None, op0=ALU.is_ge), [cst], [impb])
                op("dve", lambda e: e.tensor_tensor(out=impb[:, 1, :], in0=impb[:, 1, :], in1=cf("col0"), op=ALU.max), [impb, cst], [impb])
                op("dve", lambda e: e.scalar_tensor_tensor(out=impb[:, 2, :], in0=impb[:, 1, :], scalar=1e4, in1=impb[:, 0, :], op0=ALU.mult, op1=ALU.add), [impb], [impb])
                op("dve", lambda e: e.tensor_scalar(out=impb[:, 3, :], in0=cf("jb"), scalar1=float(2 * i), scalar2=-1e30, op0=ALU.is_gt, op1=ALU.mult), [cst], [impb])
                op("dve", lambda e: e.tensor_tensor(out=impb[:, 2, :], in0=impb[:, 2, :], in1=impb[:, 3, :], op=ALU.add), [impb], [impb])
                op("dve", lambda e: e.max(out=m8[:, 0:8], in_=impb[:, 2, :]), [impb], [m8])
                op("dve", lambda e: e.match_replace(out=impb[:, 4, :], in_to_replace=m8[:, 0:8], in_values=impb[:, 2, :], imm_value=-3e38), [impb, m8], [impb])
                op("dve", lambda e: e.max(out=m8[:, 8:16], in_=impb[:, 4, :]), [impb], [m8])
                op("dve", lambda e: e.memset(nselb[:], 0.0), [], [nselb])
                op("dve", lambda e: e.tensor_scalar(out=nselb[:, 0:NSEL], in0=impb[:, 2, :], scalar1=m8[:, 15:16], scalar2=NEG, op0=ALU.is_lt, op1=ALU.mult), [impb, m8], [nselb])
                op("pe", lambda e: e.transpose(out=psb[1][:, 128:256], in_=nselb[:], identity=identb[:]), [nselb, identb], [psb[1]])
                op("act", lambda e, g=g: e.copy(out=nselT[:, g, :].rearrange("p (r t) -> p r t", t=128), in_=psb[1].ap(128, [[0, 4], [1, 128]])), [psb[1]], [nselT])
                accs = lambda r: psf[4][:, r * 65:(r + 1) * 65]
                for kt in range(i + 1):
                    ek = Ekt[si % 4]
                    op("pool", lambda e, ek=ek, kt=kt: e.tensor_copy(out=ek[:].rearrange("p (s k) -> p s k", k=64), in_=identb.ap(2 * kt, [[1, 2], [0, 64]])), [identb], [ek])
                    masks = [(ek[:], nselT[:, g, :], [ek, nselT])]
                    if kt == i:
                        masks.append((identb[:], caus4[:], [identb, caus4]))
                    if kt == i:
                        attn_tile(kselT[:, g, :], [kselT], QrTg, masks, vsel[:, 0, g, 0:65], [vsel], accs, [psf[4]], kt == 0, kt == i, si)
                    else:
                        kr_ = kring[si % 4]
                        vr_ = vring[si % 4]
                        dma("sp", kr_[:], kselD[kt, :, g * 128:(g + 1) * 128], reads=[DR["kselD"]], writes=[kr_])
                        dma("sp", vr_[:], vselD[kt, :, g * 66:(g + 1) * 66], reads=[DR["vselD"]], writes=[vr_])
                        attn_tile(kr_[:], [kr_], QrTg, masks, vr_[:, 0:65], [vr_], accs, [psf[4]], kt == 0, kt == i, si)
                    si += 1
                accw = lambda r: psf[5][:, r * 65:(r + 1) * 65]
                k0 = max(0, i - 4)
                for kt in range(k0, i + 1):
                    masks = []
                    if kt == i:
                        masks.append((identb[:], caus4[:], [identb, caus4]))
                    elif kt == i - 4:
                        masks.append((identb[:], upper4[:], [identb, upper4]))
                    sl = kt % KWR
                    attn_tile(kwinR[:, g, sl * 128:(sl + 1) * 128], [kwinR], QrTg, masks, vwinR[:, sl, g, 0:65], [vwinR], accw, [psf[5]], kt == k0, kt == i, si)
                    si += 1
                for bi, bk in ((1, psf[4]), (2, psf[5])):
                    op("dve", lambda e, bk=bk: e.tensor_scalar(out=st8[:, 12:16], in0=bk.ap(64, [[65, 4]]), scalar1=1e-30, scalar2=None, op0=ALU.max), [bk], [st8])
                    op("dve", lambda e: e.reciprocal(out=st8[:, 12:16], in_=st8[:, 12:16]), [st8], [st8])
                    op("dve", lambda e, bi=bi, g=g: e.tensor_tensor(out=coef[:, bi, g * 4:(g + 1) * 4], in0=st8[:, 12:16], in1=gates.ap(g * 12 + bi, [[3, 4]]), op=ALU.mult), [st8, gates], [coef])
                    op("dve", lambda e, bk=bk, bi=bi, g=g: e.tensor_tensor(out=otmp[:].rearrange("p (r d) -> p r d", d=64), in0=bk.ap(0, [[65, 4], [1, 64]]), in1=coef.ap(bi * 8 + g * 4, [[1, 4], [0, 64]]), op=ALU.mult), [bk, coef], [otmp])
                    op("dve", lambda e, g=g: e.tensor_tensor(out=onsa[:, g * 256:(g + 1) * 256], in0=onsa[:, g * 256:(g + 1) * 256], in1=otmp[:], op=ALU.add), [onsa, otmp], [onsa])
            rms_rstd(onsa[:], 512, st8[:, 1:2], [onsa])
            op("dve", lambda e: e.scalar_tensor_tensor(out=ob[:, 0:512], in0=onsa[:], scalar=st8[:, 1:2], in1=NSAW, op0=ALU.mult, op1=ALU.mult), [onsa, st8, prm], [ob])

            if STG < 5:
                dma("sp", x1_d[i * 128:(i + 1) * 128, :], xti[:], reads=[xti], writes=[DR["x1"]])
                continue
            for k in range(4):
                wv = cw.ap(k, [[4, 8], [0, 128]])
                if k == 0:
                    op("dve", lambda e, wv=wv: e.tensor_tensor(out=cacc[:], in0=xbcp[:, :, 0:128], in1=wv, op=ALU.mult), [xbcp, cw], [cacc])
                else:
                    op("pool", lambda e, wv=wv, k=k: e.tensor_tensor(out=ctmp[:], in0=xbcp[:, :, k:k + 128], in1=wv, op=ALU.mult), [xbcp, cw], [ctmp])
                    op("dve", lambda e: e.tensor_tensor(out=cacc[:], in0=cacc[:], in1=ctmp[:], op=ALU.add), [cacc, ctmp], [cacc])
            op("dve", lambda e: e.tensor_tensor(out=cacc[:], in0=cacc[:], in1=cbs.ap(0, [[1, 8], [0, 128]]), op=ALU.add), [cacc, cbs], [cacc])
            op("act", lambda e: e.activation(out=xbcs[:], in_=cacc[:], func=AF.Silu), [cacc], [xbcs])
            op("pool", lambda e: e.tensor_copy(out=xbcp[:, :, 0:3], in_=xbcp[:, :, 128:131]), [xbcp], [xbcp])
            for c in range(6):
                op("pe", lambda e, c=c: e.transpose(out=psb[0][:, c * 128:(c + 1) * 128], in_=xbcs[:, c, :], identity=identb[:]), [xbcs, identb], [psb[0]])
            op("act", lambda e: e.copy(out=xstok[:], in_=psb[0][:, 0:512]), [psb[0]], [xstok])
            op("dve", lambda e: e.tensor_copy(out=Btok[:], in_=psb[0][:, 512:768].rearrange("p (g n) -> p g n", n=128)), [psb[0]], [Btok])
            op("dve", lambda e: e.tensor_tensor(out=dts[:, 0:8], in0=tok[:, TDT:TDT + 8], in1=DTB, op=ALU.add), [tok, prm], [dts])
            op("act", lambda e: e.activation(out=dts[:, 0:8], in_=dts[:, 0:8], func=AF.Exp), [dts], [dts])
            op("act", lambda e: e.activation(out=dts[:, 0:8], in_=dts[:, 0:8], func=AF.Ln, bias=1.0), [dts], [dts])
            op("dve", lambda e: e.tensor_tensor(out=dts[:, 8:16], in0=dts[:, 0:8], in1=AN, op=ALU.mult), [dts, prm], [dts])
            op("dve", lambda e: e.tensor_tensor(out=Xd[:].rearrange("p (h d) -> p h d", d=64), in0=xstok[:].rearrange("p (h d) -> p h d", d=64), in1=dts.ap(0, [[1, 8], [0, 64]]), op=ALU.mult), [xstok, dts], [Xd])
            op("dve", lambda e: e.tensor_tensor(out=atri[:], in0=dts.ap(8, [[1, 8], [0, 128]]), in1=cst.ap(cfo("tri"), [[0, 8], [1, 128]]), op=ALU.mult), [dts, cst], [atri])
            op("pe", lambda e: e.matmul(psf[2][:, 0:8], lhsT=cf("m1"), rhs=dts[:, 8:16], start=True, stop=True), [cst, dts], [psf[2]])
            op("act", lambda e: e.activation(out=dts[:, 16:24], in_=psf[2][:, 0:8], func=AF.Exp), [psf[2]], [dts])
            op("dve", lambda e: e.tensor_tensor(out=Xdd[:].rearrange("p (h d) -> p h d", d=64), in0=Xd[:].rearrange("p (h d) -> p h d", d=64), in1=dts.ap(16, [[1, 8], [0, 64]]), op=ALU.mult), [Xd, dts], [Xdd])
            for g in range(2):
                op("pe", lambda e, g=g: e.matmul(psf[3][:, g * 128:(g + 1) * 128], lhsT=xbcs[:, 4 + g, :], rhs=xbcs[:, 6 + g, :], start=True, stop=True), [xbcs], [psf[3]])
            op("dve", lambda e: e.tensor_tensor(out=cbm[:], in0=psf[3][:, 0:256].rearrange("p (g l) -> p g l", l=128), in1=cst.ap(cfo("tri"), [[0, 2], [1, 128]]), op=ALU.mult), [psf[3], cst], [cbm])
            for g in range(2):
                at_g = atri.ap(g * 512, [[1, 512]])
                op("pe", lambda e, at_g=at_g: e.matmul(psf[4][:, :], lhsT=cf("m1"), rhs=at_g, start=True, stop=True), [cst, atri], [psf[4]])
                op("pe", lambda e, at_g=at_g: e.matmul(psf[5][:, :], lhsT=cf("ones"), rhs=at_g, start=True, stop=True), [cst, atri], [psf[5]])
                op("act", lambda e: e.activation(out=lexp[:], in_=psf[4][:, :].rearrange("p (h l) -> p h l", l=128), func=AF.Exp), [psf[4]], [lexp])
                op("act", lambda e, g=g: e.activation(out=eab[:, g * 4:(g + 1) * 4, :], in_=psf[5][:, :].rearrange("p (h l) -> p h l", l=128), func=AF.Exp), [psf[5]], [eab])
                op("dve", lambda e, g=g: e.tensor_tensor(out=cbl[:, g * 4:(g + 1) * 4, :], in0=lexp[:], in1=cbm.ap(g * 128, [[0, 4], [1, 128]]), op=ALU.mult), [lexp, cbm], [cbl])
                op("pool", lambda e, g=g: e.tensor_tensor(out=cth[:, g * 4:(g + 1) * 4, :], in0=eab[:, g * 4:(g + 1) * 4, :], in1=xbcs.ap((6 + g) * 128, [[0, 4], [1, 128]]), op=ALU.mult), [eab, xbcs], [cth])
            for h in range(8):
                op("pe", lambda e, h=h: e.matmul(psf[2][:, h * 64:(h + 1) * 64], lhsT=cbl[:, h, :], rhs=Xd[:, h * 64:(h + 1) * 64], start=True, stop=False), [cbl, Xd], [psf[2]])
                op("pe", lambda e, h=h: e.matmul(psf[2][:, h * 64:(h + 1) * 64], lhsT=cth[:, h, :], rhs=hstb[:, h * 64:(h + 1) * 64], start=False, stop=True), [cth, hstb], [psf[2]])
            for g in range(2):
                op("pe", lambda e, g=g: e.matmul(psf[3][:, g * 256:(g + 1) * 256], lhsT=Btok[:, g, :], rhs=Xdd[:, g * 256:(g + 1) * 256], start=True, stop=True), [Btok, Xdd], [psf[3]])
            op("dve", lambda e: e.tensor_tensor(out=hst[:].rearrange("p (h d) -> p h d", d=64), in0=hst[:].rearrange("p (h d) -> p h d", d=64), in1=eab.ap(127, [[128, 8], [0, 64]]), op=ALU.mult), [hst, eab], [hst])
            op("dve", lambda e: e.tensor_tensor(out=hst[:], in0=hst[:], in1=psf[3][:, :], op=ALU.add), [hst, psf[3]], [hst])
            op("pool", lambda e: e.tensor_copy(out=hstb[:], in_=hst[:]), [hst], [hstb])
            op("dve", lambda e: e.tensor_tensor(out=yss[:], in0=xstok[:], in1=DSK, op=ALU.mult), [xstok, prm], [yss])
            op("dve", lambda e: e.tensor_tensor(out=yss[:], in0=yss[:], in1=psf[2][:, :], op=ALU.add), [yss, psf[2]], [yss])
            op("act", lambda e: e.activation(out=zs[:], in_=tok[:, TZ:TZ + 512], func=AF.Silu), [tok], [zs])
            op("dve", lambda e: e.tensor_tensor(out=yss[:], in0=yss[:], in1=zs[:], op=ALU.mult), [yss, zs], [yss])
            rms_rstd(yss[:], 512, st8[:, 2:3], [yss])
            op("dve", lambda e: e.scalar_tensor_tensor(out=ob[:, 512:1024], in0=yss[:], scalar=st8[:, 2:3], in1=SSDW, op0=ALU.mult, op1=ALU.mult), [yss, st8, prm], [ob])

            if STG < 6:
                dma("sp", x1_d[i * 128:(i + 1) * 128, :], xti[:], reads=[xti], writes=[DR["x1"]])
                continue
            to_featT(ob, oT, None, None, 8, [ob])
            for hh in range(2):
                for c in range(8):
                    op("pe", lambda e, c=c, hh=hh: e.matmul(psf[2 + hh][:, :], lhsT=oT[:, c, :], rhs=woutb[:, c, hh * 512:(hh + 1) * 512], start=(c == 0), stop=(c == 7)), [oT, woutb], [psf[2 + hh]])
            for hh in range(2):
                op("dve", lambda e, hh=hh: e.tensor_tensor(out=junk[:, hh * 512:(hh + 1) * 512], in0=psf[2 + hh][:, :], in1=gb[:, 0, hh * 512:(hh + 1) * 512], op=ALU.mult), [psf[2 + hh], gb], [junk])
            op("dve", lambda e: e.tensor_tensor(out=xti[:], in0=xti[:], in1=junk[:], op=ALU.add), [xti, junk], [xti])
            dma("sp", x1_d[i * 128:(i + 1) * 128, :], xti[:], reads=[xti], writes=[DR["x1"]])
            if dbg:
                dma("sp", dbg_d[i, :, 0:512], onsa[:], reads=[onsa], writes=[DR["dbg"]])
                dma("sp", dbg_d[i, :, 512:1024], yss[:], reads=[yss], writes=[DR["dbg"]])
                dma("sp", dbg_d[i, :, 1024:1024 + NSEL], impb[:, 2, :], reads=[impb], writes=[DR["dbg"]])
                dma("sp", dbg_d[i, :, 1536:2048], tok[:, TZ:TZ + 512], reads=[tok], writes=[DR["dbg"]])
                dma("sp", dbg_d[i, :, 1280:1288], dts[:, 0:8], reads=[dts], writes=[DR["dbg"]])
                dma("sp", dbg_d[i, :, 1288:1312], gates[:, :], reads=[gates], writes=[DR["dbg"]])

        kb.finish([DR["x1"], DR["dbg"]])
        kb.barrier()
        es_mix.close()
        es_mixp.close()
        kb.es = es
        build.kb = kb
        if PEERON:
            NTP = NT if NTILES is None else NTILES
            wqb = kb.sb("wqb", [128, 8, 2048], BF16)
            keysTb = kb.sb("keysTb", [128, 16, 128], BF16)
            fnwb = kb.sb("fnwb", [128, D], F32)
            LR = kb.sb("LR", [128, 16384], BF16)
            LT = kb.sb("LT", [128, 128, 128], BF16)
            RT = kb.sb("RT", [128, 128, 128], BF16)
            scb = kb.sb("scb", [128, 2048], F32)
            cand = kb.sb("cand", [128, 2048], F32)
            qnb = kb.sb("qnb", [128, 2048], BF16)
            qnT = kb.sb("qnT", [128, 16, 128], BF16)
            h2T = kb.sb("h2T", [128, 8, 128], BF16)
            pxt = kb.sb("pxt", [128, D], F32)
            pjk = kb.sb("pjk", [128, D], F32)
            pxn = kb.sb("pxn", [128, D], BF16)
            stp = kb.sb("stp", [128, 16, 16], F32)
            ctop = kb.sb("ctop", [128, 8, 16], F32)
            mrp = kb.sb("mrp", [128, 256], F32)
            sm = kb.sb("sm", [128, 512], F32)
            bh = kb.sb("bh", [128, 8, 128], F32)
            dnbuf = [kb.sb("dnbuf%d" % i_, [128, 8, 128], BF16) for i_ in range(2)]
            upbuf = [kb.sb("upbuf%d" % i_, [128, D], BF16) for i_ in range(2)]
            Gs = kb.sb("Gs", [128, 128], F32)
            GW = [kb.sb("GW%d" % i_, [128, 128], BF16) for i_ in range(2)]
            cstg = [scb, cand]
            cstb = [qnb, kb.sb("cstb1", [128, 1024], BF16)]
            dma("sp", fnwb[:], fnw_d.partition_broadcast(128), writes=[fnwb])
            qnws = kb.sb("qnws", [128, 2], F32)
            dma("sp", qnws[:], qnw_d[:, :], writes=[qnws])
            for kc in range(8):
                s_ = cstg[kc % 2]
                dma("sp", s_[:], wq_d[kc * 128:(kc + 1) * 128, :], writes=[s_])
                op("dve", lambda e, s_=s_, kc=kc: e.tensor_copy(out=wqb[:, kc, :], in_=s_[:]), [s_], [wqb])
            s_ = cstg[0]
            dma("sp", s_[:], keys_d.rearrange("p a n -> p (a n)"), writes=[s_])
            for hk in range(16):
                op("dve", lambda e, hk=hk, s_=s_: e.tensor_scalar(out=keysTb[:, hk, :], in0=s_[:, hk * 128:(hk + 1) * 128], scalar1=qnws[:, hk % 2:hk % 2 + 1], scalar2=None, op0=ALU.mult), [s_, qnws], [keysTb])
            for i2 in range(128):
                for which in range(2):
                    s_ = cstg[(i2 * 2 + which) % 2]
                    b_ = cstb[(i2 * 2 + which) % 2]
                    if which == 0:
                        dma("sp", s_[:, 0:1024], down_d[i2].rearrange("p c i -> p (c i)"), writes=[s_])
                    else:
                        dma("sp", s_[:, 0:1024], up_d[i2], writes=[s_])
                    op("dve" if which == 0 else "pool", lambda e, s_=s_, b_=b_: e.tensor_copy(out=b_[:, 0:1024], in_=s_[:, 0:1024]), [s_], [b_])
                    dma("sp", (dnb_d if which == 0 else upb_d)[i2], b_[:, 0:1024], reads=[b_], writes=[DR["dnb" if which == 0 else "upb"]])

            def top16(src_ap, dst_ap, n, src_bufs, dst_tt):
                op("dve", lambda e: e.max(out=dst_ap(0), in_=src_ap), src_bufs, [dst_tt])
                op("dve", lambda e: e.match_replace(out=mrp[:, 0:n], in_to_replace=dst_ap(0), in_values=src_ap, imm_value=-3e38), src_bufs + [dst_tt], [mrp])
                op("dve", lambda e: e.max(out=dst_ap(8), in_=mrp[:, 0:n]), [mrp], [dst_tt])

            for i in range(NTP):
                dma("sp", pxt[:], x1_d[i * 128:(i + 1) * 128, :], reads=[DR["x1"]], writes=[pxt])
                op("act", lambda e: e.activation(out=pjk[:], in_=pxt[:], func=AF.Square, accum_out=sm[:, 0:1]), [pxt], [pjk, sm])
                op("dve", lambda e: e.tensor_scalar(out=sm[:, 0:1], in0=sm[:, 0:1], scalar1=1.0 / D, scalar2=EPS, op0=ALU.mult, op1=ALU.add), [sm], [sm])
                op("act", lambda e: e.activation(out=sm[:, 0:1], in_=sm[:, 0:1], func=AF.Sqrt), [sm], [sm])
                op("dve", lambda e: e.reciprocal(out=sm[:, 0:1], in_=sm[:, 0:1]), [sm], [sm])
                op("dve", lambda e: e.tensor_scalar(out=pxn[:], in0=pxt[:], scalar1=sm[:, 0:1], scalar2=None, op0=ALU.mult), [pxt, sm], [pxn])
                for c in range(8):
                    op("pe", lambda e, c=c: e.transpose(out=psb[0][:, c * 128:(c + 1) * 128], in_=pxn[:, c * 128:(c + 1) * 128], identity=identb[:]), [pxn, identb], [psb[0]])
                for c in range(8):
                    op("act", lambda e, c=c: e.activation(out=h2T[:, c, :], in_=psb[0][:, c * 128:(c + 1) * 128], func=AF.Identity, scale=msc[:, 8 + c:9 + c], bias=modfm[:, 24 + c:25 + c]), [psb[0], msc, modfm], [h2T])
                for bq in range(4):
                    for kc in range(8):
                        op("pe", lambda e, bq=bq, kc=kc: e.matmul(psf[2 + bq][:, :], lhsT=h2T[:, kc, :], rhs=wqb[:, kc, bq * 512:(bq + 1) * 512], start=(kc == 0), stop=(kc == 7)), [h2T, wqb], [psf[2 + bq]])
                for h in range(8):
                    op("act", lambda e, h=h: e.activation(out=pjk[:, 0:256], in_=psf[2 + h // 2][:, (h % 2) * 256:(h % 2 + 1) * 256], func=AF.Square, accum_out=sm[:, 8 + h:9 + h]), [psf[2 + h // 2]], [pjk, sm])
                op("dve", lambda e: e.tensor_scalar(out=sm[:, 8:16], in0=sm[:, 8:16], scalar1=1.0 / 256, scalar2=EPS, op0=ALU.mult, op1=ALU.add), [sm], [sm])
                op("act", lambda e: e.activation(out=sm[:, 8:16], in_=sm[:, 8:16], func=AF.Sqrt), [sm], [sm])
                op("dve", lambda e: e.reciprocal(out=sm[:, 8:16], in_=sm[:, 8:16]), [sm], [sm])
                for bq in range(4):
                    op("dve", lambda e, bq=bq: e.tensor_tensor(out=qnb[:, bq * 512:(bq + 1) * 512].rearrange("p (h d) -> p h d", d=256), in0=psf[2 + bq][:, :].rearrange("p (h d) -> p h d", d=256), in1=sm.ap(8 + 2 * bq, [[1, 2], [0, 256]]), op=ALU.mult), [psf[2 + bq], sm], [qnb])
                for hk in range(16):
                    op("pe", lambda e, hk=hk: e.transpose(out=psb[hk // 8][:, (hk % 8) * 128:(hk % 8 + 1) * 128], in_=qnb[:, hk * 128:(hk + 1) * 128], identity=identb[:]), [qnb, identb], [psb[hk // 8]])
                for bb in range(2):
                    op("act" if bb == 0 else "dve", (lambda e, bb=bb: e.copy(out=qnT[:, bb * 8:(bb + 1) * 8, :], in_=psb[bb][:, :].rearrange("p (c t) -> p c t", t=128))) if bb == 0 else (lambda e, bb=bb: e.tensor_copy(out=qnT[:, bb * 8:(bb + 1) * 8, :], in_=psb[bb][:, :].rearrange("p (c t) -> p c t", t=128))), [psb[bb]], [qnT])
                for hk in range(16):
                    op("pe", lambda e, hk=hk: e.matmul(psf[2 + hk // 4][:, (hk % 4) * 128:(hk % 4 + 1) * 128], lhsT=qnT[:, hk, :], rhs=keysTb[:, hk, :], start=True, stop=True), [qnT, keysTb], [psf[2 + hk // 4]])
                for bq in range(4):
                    op("act" if bq % 2 else "dve", (lambda e, bq=bq: e.copy(out=scb[:, bq * 512:(bq + 1) * 512], in_=psf[2 + bq][:, :])) if bq % 2 else (lambda e, bq=bq: e.tensor_copy(out=scb[:, bq * 512:(bq + 1) * 512], in_=psf[2 + bq][:, :])), [psf[2 + bq]], [scb])
                for hk in range(16):
                    top16(scb[:, hk * 128:(hk + 1) * 128], lambda o, hk=hk: stp[:, hk, o:o + 8], 128, [scb], stp)
                op("dve", lambda e: e.tensor_tensor(out=cand[:].rearrange("p (h a b) -> p h a b", a=16, b=16), in0=stp.ap(0, [[32, 8], [1, 16], [0, 16]]), in1=stp.ap(16, [[32, 8], [0, 16], [1, 16]]), op=ALU.add), [stp], [cand])
                for h in range(8):
                    top16(cand[:, h * 256:(h + 1) * 256], lambda o, h=h: ctop[:, h, o:o + 8], 256, [cand], ctop)
                op("dve", lambda e: e.tensor_tensor(out=sm[:, 128:256].rearrange("p (h a) -> p h a", a=16), in0=ctop[:, :, :], in1=ctop.ap(0, [[16, 8], [0, 16]]), op=ALU.subtract), [ctop], [sm])
                op("act", lambda e: e.activation(out=sm[:, 128:256], in_=sm[:, 128:256], func=AF.Exp), [sm], [sm])
                op("dve", lambda e: e.tensor_reduce(out=sm[:, 16:24], in_=sm[:, 128:256].rearrange("p (h a) -> p h a", a=16), axis=AX.X, op=ALU.add), [sm], [sm])
                op("dve", lambda e: e.reciprocal(out=sm[:, 16:24], in_=sm[:, 16:24]), [sm], [sm])
                op("dve", lambda e: e.tensor_tensor(out=sm[:, 256:384].rearrange("p (h a) -> p h a", a=16), in0=stp.ap(0, [[32, 8], [1, 16]]), in1=stp.ap(0, [[32, 8], [0, 16]]), op=ALU.subtract), [stp], [sm])
                op("act", lambda e: e.activation(out=sm[:, 256:384], in_=sm[:, 256:384], func=AF.Exp), [sm], [sm])
                op("dve", lambda e: e.tensor_tensor(out=sm[:, 384:512].rearrange("p (h a) -> p h a", a=16), in0=ctop.ap(15, [[16, 8], [0, 16]]), in1=stp.ap(0, [[32, 8], [1, 16]]), op=ALU.subtract), [ctop, stp], [sm])
                op("dve", lambda e: e.tensor_tensor(out=bh[:], in0=scb.ap(128, [[256, 8], [1, 128]]), in1=stp.ap(16, [[32, 8], [0, 128]]), op=ALU.subtract), [scb, stp], [bh])
                op("act", lambda e: e.activation(out=bh[:], in_=bh[:], func=AF.Exp), [bh], [bh])
                op("dve", lambda e: e.tensor_tensor(out=bh[:], in0=bh[:], in1=sm.ap(16, [[1, 8], [0, 128]]), op=ALU.mult), [bh, sm], [bh])
                LRv = LR[:].rearrange("p (h a n) -> p h a n", a=16, n=128)
                for side in range(2):
                    if side == 0:
                        op("dve", lambda e: e.tensor_tensor(out=LRv, in0=scb.ap(0, [[256, 8], [0, 16], [1, 128]]), in1=stp.ap(0, [[32, 8], [1, 16], [0, 128]]), op=ALU.is_equal), [scb, stp], [LR])
                        op("pool", lambda e: e.tensor_tensor(out=LRv, in0=LRv, in1=sm.ap(256, [[16, 8], [1, 16], [0, 128]]), op=ALU.mult), [LR, sm], [LR])
                    else:
                        op("dve", lambda e: e.tensor_tensor(out=LRv, in0=scb.ap(128, [[256, 8], [0, 16], [1, 128]]), in1=sm.ap(384, [[16, 8], [1, 16], [0, 128]]), op=ALU.is_ge), [scb, sm], [LR])
                        op("pool", lambda e: e.tensor_tensor(out=LRv, in0=LRv, in1=bh.ap(0, [[128, 8], [0, 16], [1, 128]]), op=ALU.mult), [LR, bh], [LR])
                    dstT = LT if side == 0 else RT
                    for ii in range(128):
                        op("pe", lambda e, ii=ii: e.transpose(out=psb[(ii // 8) % 2][:, (ii % 8) * 128:(ii % 8 + 1) * 128], in_=LR.ap(ii, [[128, 128]]), identity=identb[:]), [LR, identb], [psb[(ii // 8) % 2]])
                        if ii % 8 == 7:
                            i0 = ii - 7
                            eng = "act" if (ii // 8) % 2 else "dve"
                            op(eng, (lambda e, i0=i0, ii=ii, dstT=dstT: e.copy(out=dstT[:, i0:i0 + 8, :], in_=psb[(ii // 8) % 2][:, :].rearrange("p (c t) -> p c t", t=128))) if eng == "act" else (lambda e, i0=i0, ii=ii, dstT=dstT: e.tensor_copy(out=dstT[:, i0:i0 + 8, :], in_=psb[(ii // 8) % 2][:, :].rearrange("p (c t) -> p c t", t=128))), [psb[(ii // 8) % 2]], [dstT])
                for t in range(128):
                    bk = psf[2 + (t // 4) % 2]
                    op("pe", lambda e, t=t, bk=bk: e.matmul(bk[:, (t % 4) * 128:(t % 4 + 1) * 128], lhsT=LT.ap(t, [[128, 128]]), rhs=RT.ap(t, [[128, 128]]), start=True, stop=True), [LT, RT], [bk])
                    if t % 4 == 3:
                        t0 = t - 3
                        eng = "act" if (t // 4) % 2 else "dve"
                        op(eng, (lambda e, t0=t0, bk=bk: e.copy(out=LR[:, t0 * 128:(t0 + 4) * 128], in_=bk[:, :])) if eng == "act" else (lambda e, t0=t0, bk=bk: e.tensor_copy(out=LR[:, t0 * 128:(t0 + 4) * 128], in_=bk[:, :])), [bk], [LR])
                for i2 in range(128):
                    dn_ = dnbuf[i2 % 2]
                    up_ = upbuf[i2 % 2]
                    dma("sp", dn_[:].rearrange("p c i -> p (c i)"), dnb_d[i2], reads=[DR["dnb"]], writes=[dn_])
                    dma("pool", up_[:], upb_d[i2], reads=[DR["upb"]], writes=[up_])
                    bk = psf[2 + i2 % 2]
                    for kc in range(8):
                        op("pe", lambda e, kc=kc, dn_=dn_, bk=bk: e.matmul(bk[:, 0:128], lhsT=dn_[:, kc, :], rhs=h2T[:, kc, :], start=(kc == 0), stop=(kc == 7)), [dn_, h2T], [bk])
                    op("act", lambda e, bk=bk: e.activation(out=Gs[:], in_=bk[:, 0:128], func=AF.Gelu_apprx_tanh), [bk], [Gs])
                    gw_ = GW[i2 % 2]
                    op("dve", lambda e, gw_=gw_, i2=i2: e.tensor_tensor(out=gw_[:], in0=Gs[:], in1=LR.ap(i2, [[128, 128]]), op=ALU.mult), [Gs, LR], [gw_])
                    for hh in range(2):
                        op("pe", lambda e, hh=hh, gw_=gw_, up_=up_, i2=i2: e.matmul(psf[4 + hh][:, :], lhsT=gw_[:], rhs=up_[:, hh * 512:(hh + 1) * 512], start=(i2 == 0), stop=(i2 == 127)), [gw_, up_], [psf[4 + hh]])
                for hh in range(2):
                    op("dve", lambda e, hh=hh: e.tensor_tensor(out=pjk[:, hh * 512:(hh + 1) * 512], in0=psf[4 + hh][:, :], in1=gb[:, 1, hh * 512:(hh + 1) * 512], op=ALU.mult), [psf[4 + hh], gb], [pjk])
                op("dve", lambda e: e.tensor_tensor(out=pxt[:], in0=pxt[:], in1=pjk[:], op=ALU.add), [pxt, pjk], [pxt])
                op("act", lambda e: e.activation(out=pjk[:], in_=pxt[:], func=AF.Square, accum_out=sm[:, 1:2]), [pxt], [pjk, sm])
                op("dve", lambda e: e.tensor_scalar(out=sm[:, 1:2], in0=sm[:, 1:2], scalar1=1.0 / D, scalar2=EPS, op0=ALU.mult, op1=ALU.add), [sm], [sm])
                op("act", lambda e: e.activation(out=sm[:, 1:2], in_=sm[:, 1:2], func=AF.Sqrt), [sm], [sm])
                op("dve", lambda e: e.reciprocal(out=sm[:, 1:2], in_=sm[:, 1:2]), [sm], [sm])
                op("dve", lambda e: e.scalar_tensor_tensor(out=pjk[:], in0=pxt[:], scalar=sm[:, 1:2], in1=fnwb[:], op0=ALU.mult, op1=ALU.mult), [pxt, sm, fnwb], [pjk])
                dma("sp", out_d[i * 128:(i + 1) * 128, :], pjk[:], reads=[pjk], writes=[DR["out"]])
            kb.finish([DR["out"]])
    return nc, cvals


def prep_core_inputs(inp, b, S, cvals):
    f = lambda a: np.ascontiguousarray(a, dtype=np.float32)
    fm = lambda v: f(np.asarray(v).reshape(-1, 128).T)
    m = {}
    m["x"] = f(inp["x"][b, :S])
    m["cT"] = fm(inp["c"][b])
    m["w_ada"] = f(inp["w_ada"][0])
    m["b_ada"] = f(inp["b_ada"][0])
    m["norm1_w"] = fm(inp["norm1_w"][0])
    m["norm2_w"] = fm(inp["norm2_w"][0])
    m["final_norm_w"] = f(inp["final_norm_w"])
    m["w_in"] = f(inp["w_in"][0])
    lidx = np.array([[4 * (j // 2) + (j % 2) + 2 * hf for hf in range(2)] for j in range(16)])
    rows = (lidx[:, :, None] * 64 + np.arange(64)[None, None, :]).reshape(-1)
    for nm in ("k", "v"):
        m["cmp_pe_" + nm] = fm(np.asarray(inp["cmp_pe_" + nm][0]).reshape(-1)[rows])
        m["cmp_w1_" + nm] = f(np.asarray(inp["cmp_w1_" + nm][0])[rows])
    m["cmp_w2_k"] = f(inp["cmp_w2_k"][0])
    m["cmp_w2_v"] = f(inp["cmp_w2_v"][0])
    m["nsa_norm_w"] = f(inp["nsa_norm_w"][0])
    cwt = np.asarray(inp["conv_w"][0])
    m["conv_w"] = f(cwt.reshape(4, 8, 128).transpose(2, 1, 0))
    m["conv_b"] = fm(inp["conv_b"][0])
    m["dt_bias"] = f(inp["dt_bias"][0])
    m["a_log"] = f(inp["a_log"][0])
    m["d_skip"] = f(np.repeat(np.asarray(inp["d_skip"][0]), 64))
    m["ssd_norm_w"] = f(inp["ssd_norm_w"][0])
    m["w_out"] = f(inp["w_out"][0])
    m["peer_wq"] = f(inp["peer_wq"][0])
    m["peer_qnorm_w"] = fm(inp["peer_qnorm_w"][0])
    sk = np.asarray(inp["peer_sub_keys"][0])
    m["peer_keysT"] = f(sk.reshape(16, 128, 128).transpose(2, 0, 1))
    dn = np.asarray(inp["peer_down"][0]).reshape(128, 128, 8, 128)
    m["peer_downT"] = f(dn.transpose(1, 3, 2, 0))
    upp = np.asarray(inp["peer_up"][0]).reshape(128, 128, D)
    m["peer_up"] = f(upp.transpose(1, 0, 2))
    m["consts"] = cvals
    return m


def kernel(**inputs):
    S = inputs["x"].shape[1]
    B = inputs["x"].shape[0]
    inp = {k: np.asarray(v) for k, v in inputs.items()}
    nc, cvals = build(S)
    in_maps = [prep_core_inputs(inp, b, S, cvals) for b in range(B)]
    res = run_bass_kernel_spmd(nc, in_maps, core_ids=list(range(B)))
    return np.stack([r["out"] for r in res.results], axis=0).astype(np.float32)
```
